# Optimizing a Trainium2 kernel written in Bass

```python
import jax, jax.numpy as jnp
from jax import lax
import numpy as np

D_MODEL = 1024
BATCH = 4
SEQ = 8192
DEPTH = 2

GRID_W = 64
CTX_LEN = 256
ROPE_BASE = 10000.0
NORM_EPS = 1e-6

RET_HEADS = 4
RET_DK = 64
RET_DV = 64
RET_CHUNK = 128
FOURIER_GROUPS = 4
FOURIER_GROUP_DIM = 64
NA_HEADS = 4
NA_HEAD_DIM = 64
NA_WIN_R = 8
NA_WIN_C = 16
MLA_HEADS = 4
MLA_NOPE = 64
MLA_ROPE = 32
MLA_V = 64
MLA_Q_RANK = 192
MLA_KV_RANK = 128
N_BRANCHES = 4
BRANCH_WIDTH = 256
Q_BLOCK = 128
FFN_DIM = 2816
MOE_EXPERTS = 8
MOE_TOP_K = 2
MOE_FFN_DIM = 3584
MOE_BLOCK = 256

KV_SIZES = (RET_HEADS * RET_DK, RET_HEADS * RET_DV, NA_HEADS * NA_HEAD_DIM, NA_HEADS * NA_HEAD_DIM,
            MLA_KV_RANK, MLA_ROPE)
Q_SIZES = (RET_HEADS * RET_DK, RET_HEADS * RET_DV, RET_HEADS * RET_DV,
           FOURIER_GROUPS * FOURIER_GROUP_DIM, NA_HEADS * NA_HEAD_DIM, MLA_Q_RANK,
           N_BRANCHES * D_MODEL)
KV_COLS = sum(KV_SIZES)
IN_COLS = KV_COLS + sum(Q_SIZES)

kernel_name = "hybrid_diffusion_parallel_mixer_trunk"


def rmsnorm(x, g):
    xf = x.astype(jnp.float32)
    y = xf * lax.rsqrt(jnp.mean(xf * xf, axis=-1, keepdims=True) + NORM_EPS)
    return (y * g.astype(jnp.float32)).astype(x.dtype)


def split_sizes(t, sizes):
    return jnp.split(t, [int(s) for s in np.cumsum(sizes)[:-1]], axis=-1)


def split_heads(t, n_heads):
    return t.reshape(*t.shape[:-1], n_heads, -1)


def axial_rope_tables(n, rot_dim):
    t = jnp.arange(n)
    row = (t // GRID_W).astype(jnp.float32)
    col = (t % GRID_W).astype(jnp.float32)
    nf = rot_dim // 4
    inv = ROPE_BASE ** (-jnp.arange(nf, dtype=jnp.float32) / nf)
    ang = jnp.concatenate([row[:, None] * inv, col[:, None] * inv], axis=-1)
    return jnp.cos(ang), jnp.sin(ang)


def apply_rope(x, cos, sin):
    half = x.shape[-1] // 2
    x1, x2 = x[..., :half], x[..., half:]
    c, s = cos[:, None, :], sin[:, None, :]
    return jnp.concatenate([x1 * c - x2 * s, x2 * c + x1 * s], axis=-1).astype(x.dtype)


def block_attention(q, k, v, scale):
    B, T, H, dq = q.shape
    nb = T // Q_BLOCK
    qb = jnp.moveaxis(q.reshape(B, nb, Q_BLOCK, H, dq), 1, 0)

    def attend(q_blk):
        s = jnp.einsum('bqhd,bkhd->bhqk', q_blk, k).astype(jnp.float32) * scale
        p = jax.nn.softmax(s, axis=-1).astype(v.dtype)
        return jnp.einsum('bhqk,bkhe->bqhe', p, v)

    o = lax.map(attend, qb)
    return jnp.moveaxis(o, 0, 1).reshape(B, T, H * v.shape[-1])


def retention_scan(q, k, v, log_gamma, s0):
    B, T, H, dk = q.shape
    dv = v.shape[-1]
    C = RET_CHUNK
    n = T // C
    qc = q.reshape(B, n, C, H, dk)
    kc = k.reshape(B, n, C, H, dk)
    vc = v.reshape(B, n, C, H, dv)
    pos = jnp.arange(C, dtype=jnp.float32)
    diff = pos[:, None] - pos[None, :]
    mask = jnp.where(diff >= 0, jnp.exp(log_gamma[:, None, None] * jnp.maximum(diff, 0.0)), 0.0)
    inner = jnp.einsum('bnhij,bnjhe->bnihe',
                       jnp.einsum('bnihd,bnjhd->bnhij', qc, kc) * mask, vc)
    k_w = jnp.exp(log_gamma[None, :] * (C - 1.0 - pos)[:, None])
    d_state = jnp.einsum('bnjhd,bnjhe->nbhde', kc * k_w[:, :, None], vc)
    chunk_decay = jnp.exp(log_gamma * C)[None, :, None, None]

    def step(s_prev, ds):
        return chunk_decay * s_prev + ds, s_prev

    s_final, s_before = lax.scan(step, s0, d_state)
    q_w = jnp.exp(log_gamma[None, :] * (pos + 1.0)[:, None])
    cross = jnp.einsum('bnihd,nbhde->bnihe', qc * q_w[:, :, None], s_before)
    return (inner + cross).reshape(B, T, H, dv), s_final


def retention_state(k, v, log_gamma):
    T = k.shape[1]
    w = jnp.exp(log_gamma[None, :] * (T - 1.0 - jnp.arange(T, dtype=jnp.float32))[:, None])
    return jnp.einsum('bthd,bthe->bhde', k * w[:, :, None], v)


def head_rms(o):
    o = o * lax.rsqrt(jnp.mean(o * o, axis=-1, keepdims=True) + NORM_EPS)
    return o.reshape(*o.shape[:2], -1)


def gate_directions(o_f, o_b, g_f, g_b):
    dt = g_f.dtype
    return jax.nn.silu(g_f) * head_rms(o_f).astype(dt) + jax.nn.silu(g_b) * head_rms(o_b).astype(dt)


def retention_branch(q, k, v, g_f, g_b, qc, kc, vc, gc_f, gc_b, decay_f, decay_b, rope):
    f32 = jnp.float32
    cos, sin = rope
    lg_f = jax.nn.log_sigmoid(decay_f.astype(f32))
    lg_b = jax.nn.log_sigmoid(decay_b.astype(f32))
    scale = RET_DK ** -0.5
    q = apply_rope(split_heads(q, RET_HEADS).astype(f32), cos, sin)
    k = apply_rope(split_heads(k, RET_HEADS).astype(f32), cos, sin) * scale
    v = split_heads(v, RET_HEADS).astype(f32)
    kc = split_heads(kc, RET_HEADS).astype(f32) * scale
    vc = split_heads(vc, RET_HEADS).astype(f32)
    flip = lambda t: jnp.flip(t, axis=1)
    if qc is None:
        s_f = retention_state(kc, vc, lg_f)
        s_b = retention_state(flip(kc), flip(vc), lg_b)
        yc = None
    else:
        zero = jnp.zeros((q.shape[0], RET_HEADS, RET_DK, RET_DV), f32)
        qc = split_heads(qc, RET_HEADS).astype(f32)
        oc_f, s_f = retention_scan(qc, kc, vc, lg_f, zero)
        oc_b, s_b = retention_scan(flip(qc), flip(kc), flip(vc), lg_b, zero)
        yc = gate_directions(oc_f, flip(oc_b), gc_f, gc_b)
    o_f, _ = retention_scan(q, k, v, lg_f, s_f)
    o_b, _ = retention_scan(flip(q), flip(k), flip(v), lg_b, s_b)
    y = gate_directions(o_f, flip(o_b), g_f, g_b)
    return y, yc


def fourier_mix(f):
    B, T, _ = f.shape
    fg = f.astype(jnp.float32).reshape(B, T, FOURIER_GROUPS, FOURIER_GROUP_DIM)
    spec = jnp.fft.fft2(fg, axes=(1, 3), norm='ortho')
    return jnp.real(spec).reshape(B, T, -1).astype(f.dtype)


def na_branch(q, k, v, qc, kc, vc, rpb):
    B, N, _ = q.shape
    rows = N // GRID_W
    wr = min(NA_WIN_R, rows)
    wc = NA_WIN_C
    n_loc = wr * wc
    scale = NA_HEAD_DIM ** -0.5
    qg = q.reshape(B, rows, GRID_W, NA_HEADS, NA_HEAD_DIM)
    kg = k.reshape(B, rows, GRID_W, NA_HEADS, NA_HEAD_DIM)
    vg = v.reshape(B, rows, GRID_W, NA_HEADS, NA_HEAD_DIM)
    kc = split_heads(kc, NA_HEADS)
    vc = split_heads(vc, NA_HEADS)
    cols = jnp.arange(GRID_W)
    col_idx = jnp.clip(cols - wc // 2, 0, GRID_W - wc)[:, None] + jnp.arange(wc)[None, :]
    col_bias_idx = col_idx - cols[:, None] + (NA_WIN_C - 1)
    rpb_f = rpb.astype(jnp.float32)

    def row_block(r):
        rs = jnp.clip(r - wr // 2, 0, rows - wr)
        q_r = lax.dynamic_index_in_dim(qg, r, axis=1, keepdims=False)
        k_r = lax.dynamic_slice_in_dim(kg, rs, wr, axis=1)[:, :, col_idx]
        v_r = lax.dynamic_slice_in_dim(vg, rs, wr, axis=1)[:, :, col_idx]
        k_r = jnp.moveaxis(k_r, 2, 1).reshape(B, GRID_W, n_loc, NA_HEADS, NA_HEAD_DIM)
        v_r = jnp.moveaxis(v_r, 2, 1).reshape(B, GRID_W, n_loc, NA_HEADS, NA_HEAD_DIM)
        row_bias_idx = rs + jnp.arange(wr) - r + (NA_WIN_R - 1)
        bias = rpb_f[:, row_bias_idx][:, :, col_bias_idx]
        bias = bias.transpose(0, 2, 1, 3).reshape(NA_HEADS, GRID_W, n_loc)
        s_loc = jnp.einsum('bqhd,bqkhd->bhqk', q_r, k_r).astype(jnp.float32) * scale + bias
        s_ctx = jnp.einsum('bqhd,bkhd->bhqk', q_r, kc).astype(jnp.float32) * scale
        p = jax.nn.softmax(jnp.concatenate([s_loc, s_ctx], axis=-1), axis=-1).astype(v.dtype)
        return (jnp.einsum('bhqk,bqkhd->bqhd', p[..., :n_loc], v_r)
                + jnp.einsum('bhqk,bkhd->bqhd', p[..., n_loc:], vc))

    o = lax.map(row_block, jnp.arange(rows))
    y = jnp.moveaxis(o, 0, 1).reshape(B, N, NA_HEADS * NA_HEAD_DIM)
    yc = None if qc is None else block_attention(split_heads(qc, NA_HEADS), kc, vc, scale)
    return y, yc


def mla_keys(c_kv, k_rope, kv_norm, w_ukv, rope):
    B, T, _ = c_kv.shape
    kv = split_heads(rmsnorm(c_kv, kv_norm) @ w_ukv, MLA_HEADS)
    k_nope, v = kv[..., :MLA_NOPE], kv[..., MLA_NOPE:]
    k_rope = k_rope[:, :, None, :]
    if rope is not None:
        k_rope = apply_rope(k_rope, *rope)
    k = jnp.concatenate([k_nope, jnp.broadcast_to(k_rope, (B, T, MLA_HEADS, MLA_ROPE))], axis=-1)
    return k, v


def mla_queries(c_q, q_norm, w_uq, rope):
    q = split_heads(rmsnorm(c_q, q_norm) @ w_uq, MLA_HEADS)
    q_nope, q_rope = q[..., :MLA_NOPE], q[..., MLA_NOPE:]
    if rope is not None:
        q_rope = apply_rope(q_rope, *rope)
    return jnp.concatenate([q_nope, q_rope], axis=-1)


def mla_branch(c_q, c_kv, k_r, cc_q, cc_kv, ck_r, q_norm, kv_norm, w_uq, w_ukv, rope):
    scale = (MLA_NOPE + MLA_ROPE) ** -0.5
    k, v = mla_keys(c_kv, k_r, kv_norm, w_ukv, rope)
    kc, vc = mla_keys(cc_kv, ck_r, kv_norm, w_ukv, None)
    q = mla_queries(c_q, q_norm, w_uq, rope)
    y = block_attention(q, jnp.concatenate([k, kc], axis=1), jnp.concatenate([v, vc], axis=1), scale)
    yc = None if cc_q is None else block_attention(mla_queries(cc_q, q_norm, w_uq, None), kc, vc, scale)
    return y, yc


def merge_branches(branches, gate_cols, w_branch, w_out):
    B, T, _ = gate_cols.shape
    g = jax.nn.sigmoid(gate_cols.reshape(B, T, N_BRANCHES, D_MODEL))
    m = g[:, :, 0] * (branches[0] @ w_branch[0])
    for i in range(1, N_BRANCHES):
        m = m + g[:, :, i] * (branches[i] @ w_branch[i])
    return m @ w_out


def mixer_sublayer(h, hc, w_in, decay_f, decay_b, q_norm, kv_norm, w_uq, w_ukv, rpb, w_branch, w_out,
                   ret_rope, mla_rope, ctx_out):
    u = h @ w_in
    r_k, r_v, n_k, n_v, m_ckv, m_kr = split_sizes(u[..., :KV_COLS], KV_SIZES)
    r_q, r_gf, r_gb, f_in, n_q, m_cq, gate = split_sizes(u[..., KV_COLS:], Q_SIZES)
    if ctx_out:
        uc = hc @ w_in
        c_qside = split_sizes(uc[..., KV_COLS:], Q_SIZES)
    else:
        uc = hc @ w_in[:, :KV_COLS]
        c_qside = (None,) * len(Q_SIZES)
    cr_k, cr_v, cn_k, cn_v, cm_ckv, cm_kr = split_sizes(uc[..., :KV_COLS], KV_SIZES)
    cr_q, cr_gf, cr_gb, cf_in, cn_q, cm_cq, cgate = c_qside

    y_ret, yc_ret = retention_branch(r_q, r_k, r_v, r_gf, r_gb, cr_q, cr_k, cr_v, cr_gf, cr_gb,
                                     decay_f, decay_b, ret_rope)
    y_four = fourier_mix(f_in)
    y_na, yc_na = na_branch(n_q, n_k, n_v, cn_q, cn_k, cn_v, rpb)
    y_mla, yc_mla = mla_branch(m_cq, m_ckv, m_kr, cm_cq, cm_ckv, cm_kr, q_norm, kv_norm, w_uq, w_ukv, mla_rope)
    y = merge_branches((y_ret, y_four, y_na, y_mla), gate, w_branch, w_out)
    if not ctx_out:
        return y, None
    yc = merge_branches((yc_ret, fourier_mix(cf_in), yc_na, yc_mla), cgate, w_branch, w_out)
    return y, yc


def swiglu(h, w_gate, w_up, w_down):
    return (jax.nn.silu(h @ w_gate) * (h @ w_up)) @ w_down


def moe_swiglu(h, router, w_gate, w_up, w_down):
    B, T, D = h.shape
    hf = h.reshape(-1, D)
    n_tok = hf.shape[0]
    logits = (hf @ router).astype(jnp.float32)
    top_logit, top_idx = lax.top_k(logits, MOE_TOP_K)
    top_w = jax.nn.softmax(top_logit, axis=-1)
    n_assign = n_tok * MOE_TOP_K
    exp_flat = top_idx.reshape(-1)
    tok_flat = jnp.repeat(jnp.arange(n_tok, dtype=jnp.int32), MOE_TOP_K)
    order = jnp.argsort(exp_flat)
    exp_sorted = exp_flat[order]
    tok_sorted = tok_flat[order]
    w_sorted = top_w.reshape(-1)[order]
    counts = jnp.bincount(exp_flat, length=MOE_EXPERTS)
    padded = (counts + MOE_BLOCK - 1) // MOE_BLOCK * MOE_BLOCK
    pad_end = jnp.cumsum(padded)
    pad_start = pad_end - padded
    grp_start = jnp.cumsum(counts) - counts
    dest = pad_start[exp_sorted] + jnp.arange(n_assign) - grp_start[exp_sorted]
    n_rows = -(-n_assign // MOE_BLOCK) * MOE_BLOCK + MOE_EXPERTS * MOE_BLOCK
    n_blocks = n_rows // MOE_BLOCK
    row_tok = jnp.full((n_rows,), n_tok, jnp.int32).at[dest].set(tok_sorted)
    h_pad = jnp.concatenate([hf, jnp.zeros((1, D), hf.dtype)], axis=0)
    xb = h_pad[row_tok].reshape(n_blocks, MOE_BLOCK, D)
    blk_start = jnp.arange(n_blocks) * MOE_BLOCK
    blk_exp = jnp.minimum(jnp.sum(pad_end[None, :] <= blk_start[:, None], axis=1), MOE_EXPERTS - 1)

    def expert_block(args):
        x_blk, e = args
        return (jax.nn.silu(x_blk @ w_gate[e]) * (x_blk @ w_up[e])) @ w_down[e]

    yb = lax.map(expert_block, (xb, blk_exp)).reshape(n_rows, D)
    y_assign = yb[dest] * w_sorted[:, None].astype(yb.dtype)
    out = jnp.zeros_like(hf).at[tok_sorted].add(y_assign)
    return out.reshape(B, T, D)


def setup_inputs(seed: int = 0) -> dict:
    key = jax.random.key(seed)
    ks = iter(jax.random.split(key, 32))

    def nrm(shape, std):
        return jax.random.normal(next(ks), shape, jnp.float32) * std

    n_dense = (DEPTH + 1) // 2
    n_moe = DEPTH // 2
    d_in = D_MODEL ** -0.5
    decay0 = jnp.asarray(np.log(2.0 ** (5.0 + np.arange(RET_HEADS)) - 1.0), jnp.float32)
    return {
        'x': nrm((BATCH, SEQ, D_MODEL), 1.0),
        'c': nrm((BATCH, D_MODEL), 1.0),
        'ctx': nrm((BATCH, CTX_LEN, D_MODEL), 1.0),
        'c_ctx': nrm((D_MODEL,), 1.0),
        'ada_w': nrm((DEPTH, D_MODEL, 6 * D_MODEL), 0.5 * d_in),
        'ada_b': nrm((DEPTH, 6 * D_MODEL), 0.01),
        'norm_mix': 1.0 + nrm((DEPTH, D_MODEL), 0.01),
        'norm_ffn': 1.0 + nrm((DEPTH, D_MODEL), 0.01),
        'w_in': nrm((DEPTH, D_MODEL, IN_COLS), d_in),
        'ret_decay_fwd': decay0[None, :] + nrm((DEPTH, RET_HEADS), 0.01),
        'ret_decay_bwd': decay0[None, :] + nrm((DEPTH, RET_HEADS), 0.01),
        'mla_q_norm': 1.0 + nrm((DEPTH, MLA_Q_RANK), 0.01),
        'mla_kv_norm': 1.0 + nrm((DEPTH, MLA_KV_RANK), 0.01),
        'mla_w_uq': nrm((DEPTH, MLA_Q_RANK, MLA_HEADS * (MLA_NOPE + MLA_ROPE)), MLA_Q_RANK ** -0.5),
        'mla_w_ukv': nrm((DEPTH, MLA_KV_RANK, MLA_HEADS * (MLA_NOPE + MLA_V)), MLA_KV_RANK ** -0.5),
        'na_rpb': nrm((DEPTH, NA_HEADS, 2 * NA_WIN_R - 1, 2 * NA_WIN_C - 1), 0.02),
        'w_branch': nrm((DEPTH, N_BRANCHES, BRANCH_WIDTH, D_MODEL), BRANCH_WIDTH ** -0.5),
        'w_out': nrm((DEPTH, D_MODEL, D_MODEL), d_in),
        'ffn_w_gate': nrm((n_dense, D_MODEL, FFN_DIM), d_in),
        'ffn_w_up': nrm((n_dense, D_MODEL, FFN_DIM), d_in),
        'ffn_w_down': nrm((n_dense, FFN_DIM, D_MODEL), FFN_DIM ** -0.5),
        'moe_router': nrm((n_moe, D_MODEL, MOE_EXPERTS), d_in),
        'moe_w_gate': nrm((n_moe, MOE_EXPERTS, D_MODEL, MOE_FFN_DIM), d_in),
        'moe_w_up': nrm((n_moe, MOE_EXPERTS, D_MODEL, MOE_FFN_DIM), d_in),
        'moe_w_down': nrm((n_moe, MOE_EXPERTS, MOE_FFN_DIM, D_MODEL), MOE_FFN_DIM ** -0.5),
        'norm_final': 1.0 + nrm((D_MODEL,), 0.01),
    }


def reference(x, c, ctx, c_ctx, ada_w, ada_b, norm_mix, norm_ffn, w_in, ret_decay_fwd, ret_decay_bwd,
              mla_q_norm, mla_kv_norm, mla_w_uq, mla_w_ukv, na_rpb, w_branch, w_out,
              ffn_w_gate, ffn_w_up, ffn_w_down, moe_router, moe_w_gate, moe_w_up, moe_w_down, norm_final):
    n_lat = x.shape[1]
    ret_rope = axial_rope_tables(n_lat, RET_DK)
    mla_rope = axial_rope_tables(n_lat, MLA_ROPE)
    xc = ctx
    for i in range(DEPTH):
        ctx_out = i < DEPTH - 1
        mod = (jax.nn.silu(c) @ ada_w[i] + ada_b[i])[:, None, :]
        mod_c = (jax.nn.silu(c_ctx) @ ada_w[i] + ada_b[i])[None, None, :]
        sh1, sc1, g1, sh2, sc2, g2 = jnp.split(mod, 6, axis=-1)
        csh1, csc1, cg1, csh2, csc2, cg2 = jnp.split(mod_c, 6, axis=-1)

        h = rmsnorm(x, norm_mix[i]) * (1 + sc1) + sh1
        hc = rmsnorm(xc, norm_mix[i]) * (1 + csc1) + csh1
        y, yc = mixer_sublayer(h, hc, w_in[i], ret_decay_fwd[i], ret_decay_bwd[i], mla_q_norm[i],
                               mla_kv_norm[i], mla_w_uq[i], mla_w_ukv[i], na_rpb[i], w_branch[i], w_out[i],
                               ret_rope, mla_rope, ctx_out)
        x = x + g1 * y

        j = i // 2
        if i % 2 == 0:
            ffn = lambda t: swiglu(t, ffn_w_gate[j], ffn_w_up[j], ffn_w_down[j])
        else:
            ffn = lambda t: moe_swiglu(t, moe_router[j], moe_w_gate[j], moe_w_up[j], moe_w_down[j])
        x = x + g2 * ffn(rmsnorm(x, norm_ffn[i]) * (1 + sc2) + sh2)
        if ctx_out:
            xc = xc + cg1 * yc
            xc = xc + cg2 * ffn(rmsnorm(xc, norm_ffn[i]) * (1 + csc2) + csh2)
    return rmsnorm(x, norm_final)
```

```python
import numpy as np
import ml_dtypes
import concourse.bass as bass
import concourse.mybir as mybir
from concourse.bass_utils import run_bass_kernel_spmd
from contextlib import ExitStack

F32 = mybir.dt.float32
BF16 = mybir.dt.bfloat16
AF = mybir.ActivationFunctionType
ALU = mybir.AluOpType
AX = mybir.AxisListType
NPBF = ml_dtypes.bfloat16

D = 1024
NTOK = 8192
NOWN = 4096
NCTX = 256
EPS = 1e-6
KV_COLS = 1184
IN_COLS = 6752


class Buf:
    __slots__ = ("w", "r", "psum")

    def __init__(self, psum=False):
        self.w = []
        self.r = []
        self.psum = psum


class Sched:
    NDMA = 8
    LIM = 1900

    def __init__(self, nc, es):
        self.nc = nc
        self.es = es
        self.eng = {"pe": nc.tensor, "act": nc.scalar, "dve": nc.vector,
                    "pool": nc.gpsimd, "sp": nc.sync}
        self.sems = {}
        self.cnt = {}
        self.cur = {}
        self.epoch = {}
        self.dq = {q: 0 for q in ("sp", "act", "pool")}
        self.known = {e: {} for e in self.eng}
        self.n = 0

    def _stream(self, st, inc):
        k = self.cur.get(st)
        if k is None or self.cnt[k] + inc > self.LIM:
            ep = self.epoch.get(st, -1) + 1
            self.epoch[st] = ep
            k = "%s_%d" % (st, ep)
            self.sems[k] = self.es.enter_context(self.nc.semaphore(k))
            self.cnt[k] = 0
            self.cur[st] = k
        return k

    def _wait(self, e, ev):
        k, v, _ = ev
        if self.known[e].get(k, 0) >= v:
            return
        self.eng[e].wait_ge(self.sems[k], v)
        self.known[e][k] = v

    def _deps(self, e, tag, reads, writes):
        for b in reads:
            for ev in b.w:
                self._wait(e, ev)
            if b.psum:
                for ev in b.r:
                    if ev[2] != tag:
                        self._wait(e, ev)
        for b in writes:
            for ev in b.w:
                if not (ev[2] == tag and (tag == "pe" or tag.startswith("dma_"))):
                    self._wait(e, ev)
            for ev in b.r:
                if not (ev[2] == tag and tag == "pe"):
                    self._wait(e, ev)

    def _record(self, ev, reads, writes, append=False):
        for b in reads:
            b.r.append(ev)
            if len(b.r) > 16:
                d = {}
                for x in b.r:
                    if x[0] not in d or d[x[0]][1] < x[1]:
                        d[x[0]] = x
                b.r = list(d.values())
        for b in writes:
            if append and b.w and all(x[2] == ev[2] for x in b.w):
                b.w.append(ev)
                if len(b.w) > 16:
                    d = {}
                    for x in b.w:
                        if x[0] not in d or d[x[0]][1] < x[1]:
                            d[x[0]] = x
                    b.w = list(d.values())
            else:
                b.w = [ev]
            b.r = []

    def _signal(self, e, ins):
        k = self._stream("s_" + e, 1)
        self.cnt[k] += 1
        ins.then_inc(self.sems[k], 1)
        return (k, self.cnt[k], e)

    def op(self, e, fn, reads=(), writes=(), **kw):
        self._deps(e, e, reads, writes)
        ins = getattr(self.eng[e], fn)(**kw)
        self._record(self._signal(e, ins), reads, writes)
        self.n += 1
        return ins

    def mm(self, reads, writes, last, **kw):
        e = "pe"
        self._deps(e, e, reads, writes)
        ins = self.nc.tensor.matmul(**kw)
        self.n += 1
        if last:
            ev = self._signal(e, ins)
        else:
            k = self._stream("s_pe", 1)
            ev = (k, self.cnt[k] + 1, e)
        self._record(ev, reads, writes)
        return ins

    def dma(self, q, out, in_, reads=(), writes=(), **kw):
        i = self.dq[q]
        self.dq[q] = i + 1
        st = "d_%s%d" % (q, i % self.NDMA)
        prev = self.cur.get(st)
        if prev is not None and self.cnt[prev] > 0:
            self._wait(q, (prev, self.cnt[prev], q))
        k = self._stream(st, 16)
        tag = "dma_" + q
        self._deps(q, tag, reads, writes)
        ins = self.eng[q].dma_start(out=out, in_=in_, **kw)
        self.cnt[k] += 16
        ins.then_inc(self.sems[k], 16)
        self._record((k, self.cnt[k], tag), reads, writes, append=True)
        self.n += 1
        return ins

    def barrier(self, engines=("pe", "act", "dve", "pool", "sp")):
        for e in engines:
            for k, v in self.cnt.items():
                if v > 0:
                    self._wait(e, (k, v, "x"))


_UID = [0]


def _uid():
    _UID[0] += 1
    return "_u%d" % _UID[0]


class Ring:
    def __init__(self, nc, es, name, shape, dtype, n, psum=False):
        self.t = []
        name = name + _uid()
        for i in range(n):
            if psum:
                t = es.enter_context(nc.psum_tensor("rp_%s%d" % (name, i), shape, dtype))
            else:
                t = es.enter_context(nc.sbuf_tensor("rs_%s%d" % (name, i), shape, dtype))
            self.t.append((t, Buf()))
        self.i = 0

    def get(self):
        x = self.t[self.i % len(self.t)]
        self.i += 1
        return x


class SubRing:
    def __init__(self, lst):
        self.t = list(lst)
        self.i = 0

    def get(self):
        x = self.t[self.i % len(self.t)]
        self.i += 1
        return x


def sb(nc, es, name, shape, dtype):
    return es.enter_context(nc.sbuf_tensor("sb_" + name + _uid(), shape, dtype)), Buf()


def _fm(v):
    v = np.asarray(v, np.float32).reshape(-1, 128)
    return np.ascontiguousarray(v.T)


def _rope_tables(pos, rot_dim):
    row = (pos // 64).astype(np.float32)
    col = (pos % 64).astype(np.float32)
    nf = rot_dim // 4
    inv = (10000.0 ** (-np.arange(nf, dtype=np.float32) / nf)).astype(np.float32)
    ang = np.concatenate([row[:, None] * inv, col[:, None] * inv], axis=-1).astype(np.float32)
    return np.cos(ang).astype(np.float32), np.sin(ang).astype(np.float32)


def _consts(s):
    c = {}
    own = np.arange(NOWN) + NOWN * s
    oth = np.arange(NOWN) + NOWN * (1 - s)
    pos = np.concatenate([own, oth])
    cs, sn = _rope_tables(pos, 64)
    d = np.arange(128) % 64
    sign = np.where(d < 32, -1.0, 1.0).astype(np.float32)
    c["tab_rc"] = np.ascontiguousarray(cs[:, d % 32].T)
    c["tab_rs"] = np.ascontiguousarray((sn[:, d % 32] * sign[None, :]).T)
    cs, sn = _rope_tables(pos, 32)
    d = np.arange(32)
    sign = np.where(d < 16, -1.0, 1.0).astype(np.float32)
    mc = np.zeros((128, NTOK), np.float32)
    ms = np.zeros((128, NTOK), np.float32)
    for r0 in (0, 64):
        mc[r0:r0 + 32] = cs[:, d % 16].T
        ms[r0:r0 + 32] = (sn[:, d % 16] * sign[None, :]).T
    c["tab_mc"] = mc
    c["tab_ms"] = ms
    p = np.zeros((128, 128), np.float32)
    for m in range(128):
        k = m + 32 if (m % 64) < 32 else m - 32
        p[k, m] = 1.0
    c["perm64"] = p
    p = np.zeros((128, 128), np.float32)
    for m in range(64, 96):
        k = m + 16 if m < 80 else m - 16
        p[k, m] = 1.0
    c["permq"] = p
    p = np.zeros((128, 128), np.float32)
    for m in range(32):
        k = m + 16 if m < 16 else m - 16
        p[k, m] = 1.0
    c["perm32"] = p
    a = np.arange(64)
    ang = 2 * np.pi * np.outer(a, a) / 64.0
    bdc = np.zeros((128, 128), np.float32)
    bds = np.zeros((128, 128), np.float32)
    for g in range(2):
        bdc[g * 64:(g + 1) * 64, g * 64:(g + 1) * 64] = np.cos(ang)
        bds[g * 64:(g + 1) * 64, g * 64:(g + 1) * 64] = np.sin(ang)
    c["bdc"] = bdc
    c["bds"] = bds
    cl = np.arange(64)
    cg = np.where(cl < 32, 32 * s + cl, 32 * (1 - s) + (cl - 32)).astype(np.float64)
    r = np.arange(128, dtype=np.float64)
    k0 = np.arange(64, dtype=np.float64)
    phi = 2 * np.pi * (cg[:, None, None] * k0[None, None, :] / 64.0 + r[None, :, None] * k0[None, None, :] / 8192.0)
    ft = np.zeros((64, 128, 2, 128), np.float32)
    ft[:, :, 0, :64] = np.cos(phi)
    ft[:, :, 0, 64:] = np.sin(phi)
    ft[:, :, 1, :64] = -np.sin(phi)
    ft[:, :, 1, 64:] = np.cos(phi)
    c["ft"] = ft.reshape(64, 128 * 256)
    k1 = (64 * s + np.arange(64)).astype(np.float64)
    ang = 2 * np.pi * np.outer(r, k1) / 128.0
    nrm = 1.0 / np.sqrt(8192.0 * 64.0)
    c["c3"] = (np.cos(ang) * nrm).astype(np.float32)
    c["s3n"] = (-np.sin(ang) * nrm).astype(np.float32)
    t = np.arange(256, dtype=np.float64)
    ang = 2 * np.pi * np.outer(t, t) / 256.0
    c["c256"] = (np.cos(ang) / 128.0).astype(np.float32).reshape(2, 128, 256).transpose(1, 0, 2).copy()
    c["s256n"] = (-np.sin(ang) / 128.0).astype(np.float32).reshape(2, 128, 256).transpose(1, 0, 2).copy()
    j = np.arange(128)[:, None].astype(np.float32)
    i = np.arange(128)[None, :].astype(np.float32)
    rt = np.zeros((128, 8, 128), np.float32)
    rt[:, 0] = np.maximum(i - j, 0)
    rt[:, 1] = (i >= j)
    rt[:, 2] = np.maximum(j - i, 0)
    rt[:, 3] = (j >= i)
    rt[:, 4] = i + 1.0
    rt[:, 5] = 128.0 - i
    rt[:, 6] = 128.0
    rt[:, 7, 0] = 127.0 - np.arange(128)
    rt[:, 7, 1] = np.arange(128)
    rt[:, 7, 2] = 4096.0 * s
    rt[:, 7, 3] = 4096.0 * (1 - s)
    rt[:, 7, 4] = float(s)
    rt[:, 7, 5] = float(1 - s)
    c["rt"] = rt.reshape(128, 1024)
    c["ident"] = np.eye(128, dtype=np.float32)
    sel = np.zeros((8, 8, 128), np.float32)
    for e in range(8):
        sel[e, e, :] = 1.0
    c["sel"] = sel.reshape(8, 1024)
    return c


def _na_table(rpb, s):
    out = np.full((3, 4, 16, 64, 8, 64), -30000.0, np.float32)
    for ty in range(3):
        jblk = (0, 3, 7)[ty]
        R0 = 64 * s + 8 * jblk
        a = np.arange(8)
        qr = R0 + a
        rs = np.clip(qr - 4, 0, 120)
        qc = np.arange(64)
        cs = np.clip(qc - 8, 0, 48)
        beta = np.arange(16)
        kr = R0 - 4 + beta
        kc = np.arange(64)
        vr = (kr[:, None] >= rs[None, :]) & (kr[:, None] <= rs[None, :] + 7) & (kr[:, None] >= 0) & (kr[:, None] < 128)
        vc = (kc[:, None] >= cs[None, :]) & (kc[:, None] <= cs[None, :] + 15)
        ri = np.clip(kr[:, None] - qr[None, :] + 7, 0, 14)
        ci = np.clip(kc[:, None] - qc[None, :] + 15, 0, 30)
        g = rpb[:, ri[:, None, :, None], ci[None, :, None, :]]
        valid = vr[:, None, :, None] & vc[None, :, :, None].transpose(0, 1, 3, 2) if False else (vr[:, None, :, None] & vc[None, :, None, :])
        out[ty] = np.where(valid[None], g, np.float32(-30000.0))
    return out.reshape(3, 4, 8, 128, 512)


class Ctx:
    pass


PROJ_BLOCKS = None
NOROPE = False
PHASES = ["mod", "proj", "ret", "fourier", "attn_na", "attn_mla", "out"]


def build_layer(L, debug=False):
    nc = bass.Bass("TRN2", target_bir_lowering=False)
    CTXQ = (L == 0)
    NQ = NOWN + (NCTX if CTXQ else 0)
    NKT = NTOK + NCTX
    dk = "ExternalOutput" if debug else "Internal"

    def din(name, shape, dt=F32):
        return nc.dram_tensor(name, list(shape), dt, kind="ExternalInput").ap()

    def dsc(name, shape, dt=BF16):
        return nc.dram_tensor(name, list(shape), dt, kind=dk).ap()

    g = Ctx()
    g.xT = din("xT", [D, NKT])
    g.cvec = din("cvec", [128, 16])
    g.ada_w = din("ada_w", [D, 6 * D])
    g.ada_b = din("ada_b", [128, 48])
    g.nmix = din("nmix", [128, 8])
    g.nffn = din("nffn", [128, 8])
    g.nfin = din("nfin", [128, 8])
    g.w_in = din("w_in", [D, IN_COLS])
    g.dec_bc = din("dec_bc", [128, 8])
    g.dec_fm = din("dec_fm", [128, 4])
    g.qn = din("qn", [128, 2])
    g.kvn = din("kvn", [128, 1])
    g.w_uq = din("w_uq", [192, 384])
    g.w_ukv = din("w_ukv", [128, 512])
    g.nab = din("nab", [3, 4, 8, 128, 512], BF16)
    g.w_branch = din("w_branch", [4, 256, D])
    g.w_out = din("w_out", [D, D])
    if L == 0:
        g.w_g = din("w_g", [D, 2816])
        g.w_u = din("w_u", [D, 2816])
        g.w_d = din("w_d", [2816, D])
    else:
        g.router = din("router", [D, 8])
        g.w_g = din("w_g", [8, D, 3584])
        g.w_u = din("w_u", [8, D, 3584])
        g.w_d = din("w_d", [8, 3584, D])
    for nm, shp in (("tab_rc", [128, NTOK]), ("tab_rs", [128, NTOK]), ("tab_mc", [128, NTOK]), ("tab_ms", [128, NTOK]),
                    ("perm64", [128, 128]), ("permq", [128, 128]), ("perm32", [128, 128]),
                    ("bdc", [128, 128]), ("bds", [128, 128]), ("ft", [64, 128 * 256]),
                    ("c3", [128, 64]), ("s3n", [128, 64]), ("c256", [128, 2, 256]), ("s256n", [128, 2, 256]),
                    ("rt", [128, 1024]), ("ident", [128, 128]), ("sel", [8, 1024])):
        setattr(g, nm, din(nm, shp))
    g.xoT = nc.dram_tensor("xoT", [D, NQ], F32, kind="ExternalOutput").ap()
    g.KR = dsc("KR", [256, NKT]); g.RV = dsc("RV", [NKT, 256])
    g.NK = dsc("NK", [256, NKT]); g.NV = dsc("NV", [NKT, 256])
    g.MK = dsc("MK", [4, 96, NKT]); g.MV = dsc("MV", [NKT, 256])
    g.XCS = dsc("XCS", [NKT, 512])
    g.QR = dsc("QR", [256, NQ]); g.GF = dsc("GF", [NQ, 256]); g.GB = dsc("GB", [NQ, 256])
    g.NQ_ = dsc("NQ_", [256, NQ]); g.MQ = dsc("MQ", [4, 96, NQ]); g.GT = dsc("GT", [4096, NQ])
    g.YRET = dsc("YRET", [NQ, 256]); g.YF = dsc("YF", [NQ, 256])
    g.YNA = dsc("YNA", [4, 64, NQ]); g.YMLA = dsc("YMLA", [4, 64, NQ])
    g.BP = dsc("BP", [128, 128, 256])
    g.SST = dsc("SST", [128, 2, 34, 256])
    g.MOD = dsc("MOD", [128, 96], F32)
    db = {k: Buf() for k in ("KR", "RV", "NK", "NV", "MK", "MV", "XCS", "QR", "GF", "GB", "NQ_", "MQ", "GT",
                             "YRET", "YF", "YNA", "YMLA", "BP", "SST", "xo", "MOD")}

    with ExitStack() as es0:
        S = Sched(nc, es0)
        g.S = S
        g.nc = nc
        g.db = db
        g.L = L
        g.CTXQ = CTXQ
        g.NQ = NQ
        g.NKT = NKT
        g.mod, g.bmod = sb(nc, es0, "mod", [128, 96], F32)
        g.am, g.bam = sb(nc, es0, "am", [128, 40], F32)
        g.psb = [(es0.enter_context(nc.psum_tensor("bank%d" % i, [128, 512], F32)), Buf(psum=True)) for i in range(8)]
        g.identb, g.bident = sb(nc, es0, "identb", [128, 128], BF16)
        g.ones, g.bones = sb(nc, es0, "onesb", [128, 128], BF16)
        g.identf, g.bidentf = sb(nc, es0, "identf", [128, 128], F32)
        S.dma("sp", g.identf[:], g.ident, writes=[g.bidentf])
        S.op("dve", "tensor_copy", reads=[g.bidentf], writes=[g.bident], out=g.identb[:], in_=g.identf[:])
        S.op("dve", "memset", writes=[g.bones], ap=g.ones[:], constant=1.0)
        for ph in PHASES:
            globals()["phase_" + ph](g) if "_" not in ph else phase_attn(g, ph.split("_")[1])
        S.barrier()
    return nc


def wview(ap):
    return ap.rearrange("(k p) n -> p k n", p=128)


class Caster:
    def __init__(self, g, es, n=2):
        self.g = g
        self.ring = Ring(g.nc, es, "cst", [128, 2048], F32, n)
        self.i = 0

    def load(self, dst, src, bdst, rows, shape):
        S = self.g.S
        st, bst = self.ring.get()
        n = int(np.prod(shape))
        v = st[:rows, :n]
        if len(shape) == 2:
            v = v.rearrange("p (a b) -> p a b", a=shape[0])
        S.dma("sp", v, src, writes=[bst])
        eng = "dve"
        self.i += 1
        S.op(eng, "tensor_copy", reads=[bst], writes=[bdst], out=dst, in_=v)


def phase_mod(g):
    nc, S = g.nc, g.S
    with ExitStack() as es:
        cv, bcv = sb(nc, es, "cv", [128, 16], F32)
        sc, bsc = sb(nc, es, "scv", [128, 16], F32)
        adab, badab = sb(nc, es, "adab", [128, 48], F32)
        nm, bnm = sb(nc, es, "nm", [128, 24], F32)
        wr = Ring(nc, es, "adaw", [128, 8, 512], F32, 2)
        ps_, bps = g.psb[0]
        ps = ps_[:, 0:96].rearrange("p (m w) -> p m w", w=2)
        S.dma("sp", cv[:], g.cvec, writes=[bcv])
        S.dma("sp", adab[:], g.ada_b, writes=[badab])
        S.dma("sp", nm[:, 0:8], g.nmix, writes=[bnm])
        S.dma("sp", nm[:, 8:16], g.nffn, writes=[bnm])
        S.dma("sp", nm[:, 16:24], g.nfin, writes=[bnm])
        S.op("act", "activation", reads=[bcv], writes=[bsc], out=sc[:], in_=cv[:], func=AF.Silu)
        av = wview(g.ada_w)
        for mg in range(12):
            wt, bw = wr.get()
            S.dma("sp", wt[:], av[:, :, mg * 512:(mg + 1) * 512], writes=[bw])
            for mi in range(4):
                m = mg * 4 + mi
                for k in range(8):
                    S.mm([bw, bsc], [bps], k == 7, out=ps[:, m, :], lhsT=wt[:, k, mi * 128:(mi + 1) * 128],
                         rhs=sc[:, k:16:8], start=(k == 0), stop=(k == 7))
        for w in range(2):
            S.op("dve", "tensor_tensor", reads=[bps, badab], writes=[g.bmod], out=g.mod[:, w * 48:(w + 1) * 48],
                 in0=ps[:, :, w], in1=adab[:], op=ALU.add)
        for w in range(2):
            for i, (c0, n0) in enumerate(((8, 0), (32, 8))):
                S.op("dve", "scalar_tensor_tensor", reads=[g.bmod, bnm], writes=[g.bam],
                     out=g.am[:, w * 16 + i * 8: w * 16 + i * 8 + 8], in0=g.mod[:, w * 48 + c0: w * 48 + c0 + 8],
                     scalar=1.0, in1=nm[:, n0:n0 + 8], op0=ALU.add, op1=ALU.mult)
        S.op("dve", "tensor_copy", reads=[bnm], writes=[g.bam], out=g.am[:, 32:40], in_=nm[:, 16:24])
        S.barrier()


def rstd_from_ps(g, ps, bps, out, bout, n, W, rows=128):
    S = g.S
    S.op("dve", "tensor_scalar", reads=[bps], writes=[bout], out=out[:rows, :W], in0=ps[:rows, :W],
         scalar1=1.0 / n, scalar2=EPS, op0=ALU.mult, op1=ALU.add)
    S.op("act", "activation", reads=[bout], writes=[bout], out=out[:rows, :W], in_=out[:rows, :W], func=AF.Sqrt)
    S.op("dve", "reciprocal", reads=[bout], writes=[bout], out=out[:rows, :W], in_=out[:rows, :W])


def phase_proj(g):
    nc, S, db = g.nc, g.S, g.db
    with ExitStack() as es:
        win, bwin = sb(nc, es, "win", [128, 8, IN_COLS], BF16)
        wv = wview(g.w_in)
        perm64, bp64 = sb(nc, es, "perm64", [128, 128], BF16)
        permq, bpq = sb(nc, es, "permq", [128, 128], BF16)
        perm32, bp32 = sb(nc, es, "perm32", [128, 128], BF16)
        bdc, bbdc = sb(nc, es, "bdc", [128, 256], BF16)
        wuq, bwuq = sb(nc, es, "wuq", [128, 2, 384], BF16)
        wuk, bwuk = sb(nc, es, "wuk", [128, 512], BF16)
        wuv, bwuv = sb(nc, es, "wuv", [128, 4, 64], BF16)
        nrm, bnrm = sb(nc, es, "nrm", [128, 3], F32)
        cst = Caster(g, es)
        for c0 in range(0, IN_COLS, 256):
            c1 = min(IN_COLS, c0 + 256)
            cst.load(win[:, :, c0:c1], wv[:, :, c0:c1], bwin, 128, [8, c1 - c0])
        cst.load(perm64[:], g.perm64, bp64, 128, [128])
        cst.load(permq[:], g.permq, bpq, 128, [128])
        cst.load(perm32[:], g.perm32, bp32, 128, [128])
        cst.load(bdc[:, 0:128], g.bdc, bbdc, 128, [128])
        cst.load(bdc[:, 128:256], g.bds, bbdc, 128, [128])
        cst.load(wuq[:, 0, :], g.w_uq[0:128, :], bwuq, 128, [384])
        cst.load(wuq[0:64, 1, :], g.w_uq[128:192, :], bwuq, 64, [384])
        cst.load(wuk[:], g.w_ukv, bwuk, 128, [512])
        cst.load(wuv[:], g.w_ukv.rearrange("k (h t) -> k h t", t=128)[:, :, 64:128], bwuv, 128, [4, 64])
        S.dma("sp", nrm[:, 0:2], g.qn, writes=[bnrm])
        S.dma("sp", nrm[:, 2:3], g.kvn, writes=[bnrm])
        xr = Ring(nc, es, "px", [128, 8, 512], F32, 1)
        sqr = Ring(nc, es, "psq", [128, 8, 512], BF16, 1)
        hr = Ring(nc, es, "ph", [128, 8, 512], BF16, 1)
        tr_ = Ring(nc, es, "ptmp", [128, 512], F32, 3)
        tb = Ring(nc, es, "ptb", [128, 512], BF16, 4)
        for (t_, b_) in tb.t:
            S.op("dve", "memset", writes=[b_], ap=t_[:], constant=0.0)
        ob = Ring(nc, es, "pob", [128, 512], BF16, 4)
        rtab = Ring(nc, es, "prt", [128, 2, 512], F32, 1)
        mtab = Ring(nc, es, "pmt", [128, 2, 512], F32, 1)
        lat = Ring(nc, es, "plat", [128, 2, 512], BF16, 2)
        fT = Ring(nc, es, "pfT", [128, 2, 512], BF16, 2)
        pp = SubRing(g.psb[0:7])
        rsx, brsx = sb(nc, es, "prsx", [128, 512], F32)
        xv = g.xT.rearrange("(c p) t -> p c t", p=128)

        def fm(ht, bh, c0, ncol, W):
            p, bp = pp.get()
            for k in range(8):
                S.mm([bwin, bh], [bp], k == 7, out=p[:ncol, :W], lhsT=win[:, k, c0:c0 + ncol], rhs=ht[:, k, :W],
                     start=(k == 0), stop=(k == 7))
            return p, bp

        def tm(ht, bh, c0, ncol, tt):
            p, bp = pp.get()
            for k in range(8):
                S.mm([bwin, bh], [bp], k == 7, out=p[:, :ncol], lhsT=ht[:, k, tt * 128:(tt + 1) * 128],
                     rhs=win[:, k, c0:c0 + ncol], start=(k == 0), stop=(k == 7))
            return p, bp

        def rope(p, bp, rows, W, perm, bperm, ctab, stab, btab, scale, r0=0, K=None):
            K = K or rows
            raw, braw = tb.get()
            S.op("act", "copy", reads=[bp], writes=[braw], out=raw[:K, :W], in_=p[:K, :W])
            p2, bp2 = pp.get()
            S.mm([braw, bperm], [bp2], True, out=p2[:, :W], lhsT=perm[:, :], rhs=raw[:, :W], start=True, stop=True)
            t1, bt1 = tr_.get()
            t2, bt2 = tr_.get()
            sl = slice(r0, r0 + rows)
            S.op("dve", "tensor_tensor", reads=[bp, btab, braw], writes=[bt1], out=t1[sl, :W], in0=p[sl, :W], in1=ctab[sl, :W], op=ALU.mult)
            S.op("dve", "tensor_tensor", reads=[bp2, btab], writes=[bt2], out=t2[sl, :W], in0=p2[sl, :W], in1=stab[sl, :W], op=ALU.mult)
            S.op("dve", "tensor_tensor", reads=[bt1, bt2], writes=[bt1], out=t1[sl, :W], in0=t1[sl, :W], in1=t2[sl, :W], op=ALU.add)
            o, bo = ob.get()
            S.op("act", "activation", reads=[bt1], writes=[bo], out=o[sl, :W], in_=t1[sl, :W], func=AF.Identity, scale=scale)
            return o, bo

        nblk = 17
        for blk in (PROJ_BLOCKS or range(nblk)):
            isctx = (blk == 16)
            W = 256 if isctx else 512
            t0 = blk * 512
            w = 1 if isctx else 0
            hasq = (blk < 8) or (isctx and g.CTXQ)
            q0 = t0 if blk < 8 else NOWN
            xt, bx = xr.get()
            S.dma("sp", xt[:, :, :W], xv[:, :, t0:t0 + W], writes=[bx])
            sq, bsq = sqr.get()
            S.op("act", "activation", reads=[bx], writes=[bsq], out=sq[:, :, :W], in_=xt[:, :, :W], func=AF.Square)
            p, bp = pp.get()
            for k in range(8):
                S.mm([bsq, g.bones], [bp], k == 7, out=p[:, :W], lhsT=g.ones[:], rhs=sq[:, k, :W], start=(k == 0), stop=(k == 7))
            rs_, brs = rsx, brsx
            rstd_from_ps(g, p, bp, rs_, brs, 1024.0, W)
            ht, bh = hr.get()
            for c in range(8):
                t1, bt1 = tr_.get()
                S.op("dve", "tensor_tensor", reads=[bx, brs], writes=[bt1], out=t1[:, :W], in0=xt[:, c, :W], in1=rs_[:, :W], op=ALU.mult)
                S.op("act", "activation", reads=[bt1, g.bam, g.bmod], writes=[bh], out=ht[:, c, :W], in_=t1[:, :W],
                     func=AF.Identity, scale=g.am[:, w * 16 + c: w * 16 + c + 1], bias=g.mod[:, w * 48 + c: w * 48 + c + 1])
            norope = isctx or NOROPE
            if not isctx:
                rt_, brt = rtab.get()
                S.dma("sp", rt_[:, 0, :], g.tab_rc[:, t0:t0 + 512], writes=[brt])
                S.dma("sp", rt_[:, 1, :], g.tab_rs[:, t0:t0 + 512], writes=[brt])
                mt_, bmt = mtab.get()
                S.dma("sp", mt_[:, 0, :], g.tab_mc[:, t0:t0 + 512], writes=[bmt])
                S.dma("sp", mt_[:, 1, :], g.tab_ms[:, t0:t0 + 512], writes=[bmt])
            for c in range(2):
                p, bp = fm(ht, bh, c * 128, 128, W)
                if norope:
                    o, bo = ob.get()
                    S.op("act", "activation", reads=[bp], writes=[bo], out=o[:, :W], in_=p[:, :W], func=AF.Identity, scale=0.125)
                else:
                    o, bo = rope(p, bp, 128, W, perm64, bp64, rt_[:, 0, :], rt_[:, 1, :], brt, 0.125)
                S.dma("sp", g.KR[c * 128:(c + 1) * 128, t0:t0 + W], o[:, :W], reads=[bo], writes=[db["KR"]])
            for tt in range(W // 128):
                for (c0, dst, key) in ((256, g.RV, "RV"), (768, g.NV, "NV")):
                    p, bp = tm(ht, bh, c0, 256, tt)
                    o, bo = ob.get()
                    S.op("act", "copy", reads=[bp], writes=[bo], out=o[:, :256], in_=p[:, :256])
                    S.dma("sp", dst[t0 + tt * 128: t0 + (tt + 1) * 128, :], o[:, :256], reads=[bo], writes=[db[key]])
            for c in range(2):
                p, bp = fm(ht, bh, 512 + c * 128, 128, W)
                o, bo = ob.get()
                S.op("act", "copy", reads=[bp], writes=[bo], out=o[:, :W], in_=p[:, :W])
                S.dma("sp", g.NK[c * 128:(c + 1) * 128, t0:t0 + W], o[:, :W], reads=[bo], writes=[db["NK"]])
            p, bp = fm(ht, bh, 1024, 128, W)
            s2, bs2 = tb.get()
            S.op("act", "activation", reads=[bp], writes=[bs2], out=s2[:, :W], in_=p[:, :W], func=AF.Square)
            p2, bp2 = pp.get()
            S.mm([bs2, g.bones], [bp2], True, out=p2[:, :W], lhsT=g.ones[:], rhs=s2[:, :W], start=True, stop=True)
            r2, br2 = tr_.get()
            rstd_from_ps(g, p2, bp2, r2, br2, 128.0, W)
            t1, bt1 = tr_.get()
            S.op("dve", "tensor_tensor", reads=[bp, br2], writes=[bt1], out=t1[:, :W], in0=p[:, :W], in1=r2[:, :W], op=ALU.mult)
            lt, blt = lat.get()
            S.op("act", "activation", reads=[bt1, bnrm], writes=[blt], out=lt[:, 0, :W], in_=t1[:, :W], func=AF.Identity, scale=nrm[:, 2:3])
            for h in range(4):
                p, bp = pp.get()
                S.mm([blt, bwuk], [bp], True, out=p[:64, :W], lhsT=wuk[:, h * 128:h * 128 + 64], rhs=lt[:, 0, :W], start=True, stop=True)
                o, bo = ob.get()
                S.op("act", "copy", reads=[bp], writes=[bo], out=o[:64, :W], in_=p[:64, :W])
                S.dma("sp", g.MK[h, 0:64, t0:t0 + W], o[:64, :W], reads=[bo], writes=[db["MK"]])
            for tt in range(W // 128):
                p, bp = pp.get()
                S.mm([blt, bwuv], [bp], True, out=p[:, :256], lhsT=lt[:, 0, tt * 128:(tt + 1) * 128],
                     rhs=wuv[:].rearrange("k h t -> k (h t)"), start=True, stop=True)
                o, bo = ob.get()
                S.op("act", "copy", reads=[bp], writes=[bo], out=o[:, :256], in_=p[:, :256])
                S.dma("sp", g.MV[t0 + tt * 128: t0 + (tt + 1) * 128, :], o[:, :256], reads=[bo], writes=[db["MV"]])
            p, bp = fm(ht, bh, 1152, 32, W)
            if norope:
                o, bo = ob.get()
                S.op("act", "copy", reads=[bp], writes=[bo], out=o[:32, :W], in_=p[:32, :W])
            else:
                o, bo = rope(p, bp, 32, W, perm32, bp32, mt_[:, 0, :], mt_[:, 1, :], bmt, 1.0)
            for h in range(4):
                S.dma("sp", g.MK[h, 64:96, t0:t0 + W], o[:32, :W], reads=[bo], writes=[db["MK"]])
            if (not isctx) or g.CTXQ:
                ft_, bft = fT.get()
                for c in range(2):
                    p, bp = fm(ht, bh, 1952 + c * 128, 128, W)
                    S.op("act", "copy", reads=[bp], writes=[bft], out=ft_[:, c, :W], in_=p[:, :W])
                for tt in range(W // 128):
                    p, bp = pp.get()
                    for c in range(2):
                        for z in range(2):
                            S.mm([bft, bbdc], [bp], True, out=p[:, z * 256 + c * 128: z * 256 + (c + 1) * 128],
                                 lhsT=ft_[:, c, tt * 128:(tt + 1) * 128], rhs=bdc[:, z * 128:(z + 1) * 128], start=True, stop=True)
                    o, bo = ob.get()
                    S.op("act", "copy", reads=[bp], writes=[bo], out=o[:, :], in_=p[:, :])
                    S.dma("sp", g.XCS[t0 + tt * 128: t0 + (tt + 1) * 128, :], o[:, :], reads=[bo], writes=[db["XCS"]])
            if not hasq:
                continue
            for c in range(2):
                p, bp = fm(ht, bh, 1184 + c * 128, 128, W)
                if norope:
                    o, bo = ob.get()
                    S.op("act", "copy", reads=[bp], writes=[bo], out=o[:, :W], in_=p[:, :W])
                else:
                    o, bo = rope(p, bp, 128, W, perm64, bp64, rt_[:, 0, :], rt_[:, 1, :], brt, 1.0)
                S.dma("sp", g.QR[c * 128:(c + 1) * 128, q0:q0 + W], o[:, :W], reads=[bo], writes=[db["QR"]])
            for tt in range(W // 128):
                p, bp = tm(ht, bh, 1440, 512, tt)
                o, bo = ob.get()
                S.op("act", "activation", reads=[bp], writes=[bo], out=o[:, :], in_=p[:, :], func=AF.Silu)
                S.dma("sp", g.GF[q0 + tt * 128: q0 + (tt + 1) * 128, :], o[:, 0:256], reads=[bo], writes=[db["GF"]])
                S.dma("sp", g.GB[q0 + tt * 128: q0 + (tt + 1) * 128, :], o[:, 256:512], reads=[bo], writes=[db["GB"]])
            for c in range(2):
                p, bp = fm(ht, bh, 2208 + c * 128, 128, W)
                o, bo = ob.get()
                S.op("act", "copy", reads=[bp], writes=[bo], out=o[:, :W], in_=p[:, :W])
                S.dma("sp", g.NQ_[c * 128:(c + 1) * 128, q0:q0 + W], o[:, :W], reads=[bo], writes=[db["NQ_"]])
            pa, bpa = fm(ht, bh, 2464, 128, W)
            pb, bpb = fm(ht, bh, 2592, 64, W)
            s2, bs2 = tb.get()
            s3, bs3 = tb.get()
            S.op("act", "activation", reads=[bpa], writes=[bs2], out=s2[:, :W], in_=pa[:, :W], func=AF.Square)
            S.op("act", "activation", reads=[bpb], writes=[bs3], out=s3[:64, :W], in_=pb[:64, :W], func=AF.Square)
            p2, bp2 = pp.get()
            S.mm([bs2, g.bones], [bp2], False, out=p2[:, :W], lhsT=g.ones[:], rhs=s2[:, :W], start=True, stop=False)
            S.mm([bs3, g.bones], [bp2], True, out=p2[:, :W], lhsT=g.ones[:64, :], rhs=s3[:64, :W], start=False, stop=True)
            r2, br2 = tr_.get()
            rstd_from_ps(g, p2, bp2, r2, br2, 192.0, W)
            lt, blt = lat.get()
            for (px, bpx, rows, ci) in ((pa, bpa, 128, 0), (pb, bpb, 64, 1)):
                t1, bt1 = tr_.get()
                S.op("dve", "tensor_tensor", reads=[bpx, br2], writes=[bt1], out=t1[:rows, :W], in0=px[:rows, :W], in1=r2[:rows, :W], op=ALU.mult)
                S.op("act", "activation", reads=[bt1, bnrm], writes=[blt], out=lt[:rows, ci, :W], in_=t1[:rows, :W], func=AF.Identity,
                     scale=nrm[:rows, ci:ci + 1])
            for h in range(4):
                p, bp = pp.get()
                S.mm([blt, bwuq], [bp], False, out=p[:96, :W], lhsT=wuq[:, 0, h * 96:(h + 1) * 96], rhs=lt[:, 0, :W], start=True, stop=False)
                S.mm([blt, bwuq], [bp], True, out=p[:96, :W], lhsT=wuq[:64, 1, h * 96:(h + 1) * 96], rhs=lt[:64, 1, :W], start=False, stop=True)
                if norope:
                    o, bo = ob.get()
                    S.op("act", "copy", reads=[bp], writes=[bo], out=o[:96, :W], in_=p[:96, :W])
                else:
                    o, bo = rope(p, bp, 32, W, permq, bpq, mt_[:, 0, :], mt_[:, 1, :], bmt, 1.0, r0=64, K=96)
                    S.op("act", "copy", reads=[bp], writes=[bo], out=o[:64, :W], in_=p[:64, :W])
                S.dma("sp", g.MQ[h, :, q0:q0 + W], o[:96, :W], reads=[bo], writes=[db["MQ"]])
            for c in range(32):
                p, bp = fm(ht, bh, 2656 + c * 128, 128, W)
                o, bo = ob.get()
                S.op("act", "activation", reads=[bp], writes=[bo], out=o[:, :W], in_=p[:, :W], func=AF.Sigmoid)
                S.dma("sp", g.GT[c * 128:(c + 1) * 128, q0:q0 + W], o[:, :W], reads=[bo], writes=[db["GT"]])
        S.barrier()


_CONST_CACHE = {}


def core_inputs(L, b, s, P, xT_b, xcT_b):
    if s not in _CONST_CACHE:
        _CONST_CACHE[s] = _consts(s)
    cst = _CONST_CACHE[s]
    m = {}
    own = slice(NOWN * s, NOWN * (s + 1))
    oth = slice(NOWN * (1 - s), NOWN * (2 - s))
    m["xT"] = np.ascontiguousarray(np.concatenate([xT_b[:, own], xT_b[:, oth], xcT_b], axis=1), dtype=np.float32)
    m["cvec"] = np.concatenate([_fm(P["c"][b]), _fm(P["c_ctx"])], axis=1)
    m["ada_w"] = np.ascontiguousarray(P["ada_w"][L])
    m["ada_b"] = _fm(P["ada_b"][L])
    m["nmix"] = _fm(P["norm_mix"][L])
    m["nffn"] = _fm(P["norm_ffn"][L])
    m["nfin"] = _fm(P["norm_final"])
    m["w_in"] = np.ascontiguousarray(P["w_in"][L])
    df, dbw = P["ret_decay_fwd"][L], P["ret_decay_bwd"][L]
    m["dec_bc"] = np.ascontiguousarray(np.tile(np.concatenate([df, dbw])[None, :], (128, 1)), dtype=np.float32)
    hh = np.arange(128) // 64
    m["dec_fm"] = np.stack([df[hh], df[2 + hh], dbw[hh], dbw[2 + hh]], axis=1).astype(np.float32)
    qn = np.zeros((128, 2), np.float32)
    qn[:, 0] = P["mla_q_norm"][L][:128]
    qn[:64, 1] = P["mla_q_norm"][L][128:]
    m["qn"] = qn
    m["kvn"] = np.ascontiguousarray(P["mla_kv_norm"][L][:, None], dtype=np.float32)
    m["w_uq"] = np.ascontiguousarray(P["mla_w_uq"][L])
    m["w_ukv"] = np.ascontiguousarray(P["mla_w_ukv"][L])
    m["nab"] = _na_table(np.asarray(P["na_rpb"][L], np.float32), s).astype(NPBF)
    m["w_branch"] = np.ascontiguousarray(P["w_branch"][L])
    m["w_out"] = np.ascontiguousarray(P["w_out"][L])
    if L == 0:
        m["w_g"] = np.ascontiguousarray(P["ffn_w_gate"][0])
        m["w_u"] = np.ascontiguousarray(P["ffn_w_up"][0])
        m["w_d"] = np.ascontiguousarray(P["ffn_w_down"][0])
    else:
        m["router"] = np.ascontiguousarray(P["moe_router"][0])
        m["w_g"] = np.ascontiguousarray(P["moe_w_gate"][0])
        m["w_u"] = np.ascontiguousarray(P["moe_w_up"][0])
        m["w_d"] = np.ascontiguousarray(P["moe_w_down"][0])
    for k, v in cst.items():
        m[k] = v
    return m


def _missing(name):
    def f(g, *a):
        raise NotImplementedError(
            "phase '%s' (retention / fourier / attention / merge+FFN) is not implemented yet: "
            "this kernel is INCOMPLETE and does not compute the module" % name)
    return f


phase_ret = _missing("ret")
phase_fourier = _missing("fourier")
phase_attn = _missing("attn")
phase_out = _missing("out")


def _run_layer(L, P, xT, xcT):
    nc = build_layer(L)
    maps = []
    for core in range(8):
        b, s = core // 2, core % 2
        maps.append(core_inputs(L, b, s, P, xT[b], xcT[b]))
    res = run_bass_kernel_spmd(nc, maps, core_ids=list(range(8)))
    xo = np.zeros((4, D, NTOK), np.float32)
    xco = np.zeros((4, D, NCTX), np.float32) if L == 0 else None
    for core in range(8):
        b, s = core // 2, core % 2
        o = np.asarray(res.results[core]["xoT"])
        xo[b][:, NOWN * s:NOWN * (s + 1)] = o[:, :NOWN]
        if L == 0 and s == 0:
            xco[b] = o[:, NOWN:]
    return xo, xco


def kernel(**inputs):
    P = {k: np.asarray(v) for k, v in inputs.items()}
    xT = np.ascontiguousarray(P["x"].transpose(0, 2, 1))
    xcT = np.ascontiguousarray(P["ctx"].transpose(0, 2, 1))
    x1T, xc1T = _run_layer(0, P, xT, xcT)
    x2T, _ = _run_layer(1, P, x1T, xc1T)
    return np.ascontiguousarray(x2T.transpose(0, 2, 1)).astype(np.float32)


def _ts(S, out, in0, s1, reads, writes, eng="act"):
    S.op("act", "activation", reads=reads, writes=writes, out=out, in_=in0, func=AF.Identity, scale=s1)


def phase_ret(g):
    nc, S, db = g.nc, g.S, g.db
    with ExitStack() as es:
        rt, brt = sb(nc, es, "rt", [128, 8, 128], F32)
        S.dma("sp", rt[:], g.rt.rearrange("p (a b) -> p a b", a=8), writes=[brt])
        dbc, bdbc = sb(nc, es, "dbc", [128, 8], F32)
        lgb, blgb = sb(nc, es, "lgb", [128, 8], F32)
        S.dma("sp", dbc[:], g.dec_bc, writes=[bdbc])
        for (src, bsrc, dst, bdst) in ((dbc, bdbc, lgb, blgb),):
            S.op("act", "activation", reads=[bsrc], writes=[bdst], out=dst[:], in_=src[:], func=AF.Exp, scale=-1.0)
            S.op("act", "activation", reads=[bdst], writes=[bdst], out=dst[:], in_=dst[:], func=AF.Ln, bias=1.0)
            _ts(S, dst[:], dst[:], -1.0, [bdst], [bdst])
        mk, bmk = sb(nc, es, "mk", [128, 2, 4, 128], F32)
        qw, bqw = sb(nc, es, "qw", [128, 2, 4, 128], F32)
        dec, bdec = sb(nc, es, "dec", [128, 2, 256], F32)
        kw, bkw = sb(nc, es, "kw", [128, 8], F32)
        af, baf = sb(nc, es, "af", [128, 8], F32)
        for dr in range(2):
            for h in range(4):
                S.op("act", "activation", reads=[brt, blgb], writes=[bmk], out=mk[:, dr, h, :], in_=rt[:, 2 * dr, :],
                     func=AF.Exp, scale=lgb[:, dr * 4 + h: dr * 4 + h + 1])
                S.op("dve", "tensor_tensor", reads=[bmk, brt], writes=[bmk], out=mk[:, dr, h, :], in0=mk[:, dr, h, :],
                     in1=rt[:, 2 * dr + 1, :], op=ALU.mult)
            for h in range(4):
                sc_ = lgb[:, dr * 4 + h: dr * 4 + h + 1]
                S.op("act", "activation", reads=[brt, blgb], writes=[bqw], out=qw[:, dr, h, :], in_=rt[:, 4 + dr, :], func=AF.Exp, scale=sc_)
                S.op("act", "activation", reads=[brt, blgb], writes=[bdec], out=dec[:, dr, h * 64:(h + 1) * 64], in_=rt[:, 6, 0:64], func=AF.Exp, scale=sc_)
            S.op("act", "activation", reads=[brt, blgb], writes=[baf], out=af[:, dr * 4: dr * 4 + 4], in_=lgb[:, dr * 4: dr * 4 + 4], func=AF.Exp,
                 scale=rt[:, 7, 2 + dr: 3 + dr])
            S.op("act", "activation", reads=[brt, blgb], writes=[bkw], out=kw[:, dr * 4: dr * 4 + 4], in_=lgb[:, dr * 4: dr * 4 + 4], func=AF.Exp,
                 scale=rt[:, 7, dr: dr + 1])
        ktr = Ring(nc, es, "rk", [64, 4, 128], BF16, 3)
        vr = Ring(nc, es, "rv", [128, 256], BF16, 3)
        kwr = Ring(nc, es, "rkw", [128, 256], BF16, 2)
        SB_, bSB = sb(nc, es, "SB", [128, 34, 2, 256], BF16)
        st = [sb(nc, es, "st%d" % i, [128, 256], F32) for i in range(2)]
        tst = [sb(nc, es, "tst%d" % i, [128, 256], F32) for i in range(2)]
        pA = SubRing(g.psb)
        krv = g.KR.rearrange("(h d) t -> d h t", d=64)
        qrv = g.QR.rearrange("(h d) t -> d h t", d=64)

        def load_kv(tok0):
            kt, bk = ktr.get()
            S.dma("sp", kt[:], krv[:, :, tok0:tok0 + 128], writes=[bk])
            vt, bv = vr.get()
            S.dma("sp", vt[:], g.RV[tok0:tok0 + 128, :], writes=[bv])
            return kt, bk, vt, bv

        def scan_step(tok0, dr, stt, bst):
            kt, bk, vt, bv = load_kv(tok0)
            p, bp = pA.get()
            for h in range(4):
                S.mm([bk, g.bident], [bp], True, out=p[:, h * 64:(h + 1) * 64], lhsT=kt[:, h, :], rhs=g.identb[:64, :64], start=True, stop=True)
            kwt, bkwt = kwr.get()
            for h in range(4):
                _ts(S, kwt[:, h * 64:(h + 1) * 64], p[:, h * 64:(h + 1) * 64], kw[:, dr * 4 + h: dr * 4 + h + 1], [bp, bkw], [bkwt])
            p2, bp2 = pA.get()
            for h in range(4):
                S.mm([bkwt, bv], [bp2], True, out=p2[:64, h * 64:(h + 1) * 64], lhsT=kwt[:, h * 64:(h + 1) * 64],
                     rhs=vt[:, h * 64:(h + 1) * 64], start=True, stop=True)
            S.op("dve", "tensor_tensor", reads=[bst, bdec], writes=[bst], out=stt[:64, :], in0=stt[:64, :], in1=dec[:64, dr, :], op=ALU.mult)
            S.op("dve", "tensor_tensor", reads=[bst, bp2], writes=[bst], out=stt[:64, :], in0=stt[:64, :], in1=p2[:64, 0:256], op=ALU.add)

        for dr in range(2):
            stt, bst = st[dr]
            tt_, btt = tst[dr]
            S.op("dve", "memset", writes=[bst], ap=stt[:], constant=0.0)
            for cc in ((0, 1) if dr == 0 else (1, 0)):
                S.op("act", "copy", reads=[bst], writes=[bSB], out=SB_[:64, 32 + cc, dr, :], in_=stt[:64, :])
                scan_step(NTOK + cc * 128, dr, stt, bst)
            S.op("dve", "memset", writes=[btt], ap=tt_[:], constant=0.0)
            for n in (range(32, 64) if dr == 0 else range(63, 31, -1)):
                scan_step(n * 128, dr, tt_, btt)
            for h in range(4):
                _ts(S, stt[:64, h * 64:(h + 1) * 64], stt[:64, h * 64:(h + 1) * 64], af[:64, dr * 4 + h: dr * 4 + h + 1], [bst, baf], [bst])
            _ts(S, tt_[:64, :], tt_[:64, :], rt[:64, 7, 4 + dr: 5 + dr], [btt, brt], [btt])
            S.op("dve", "tensor_tensor", reads=[btt, bst], writes=[bst], out=stt[:64, :], in0=stt[:64, :], in1=tt_[:64, :], op=ALU.add)
            for n in (range(32) if dr == 0 else range(31, -1, -1)):
                S.op("act", "copy", reads=[bst], writes=[bSB], out=SB_[:64, n, dr, :], in_=stt[:64, :])
                scan_step(n * 128, dr, stt, bst)

        qr_ = Ring(nc, es, "rq", [64, 4, 128], BF16, 2)
        gr = Ring(nc, es, "rg", [128, 2, 256], BF16, 2)
        Ar = Ring(nc, es, "rA", [128, 2, 512], BF16, 2)
        qwr = Ring(nc, es, "rqw", [64, 2, 4, 128], BF16, 2)
        sqr = Ring(nc, es, "rsq", [128, 512], F32, 2)
        ssr = Ring(nc, es, "rss", [128, 8], F32, 2)
        tr_ = Ring(nc, es, "rt2", [128, 2, 256], F32, 2)
        yo = Ring(nc, es, "ryo", [128, 256], BF16, 2)
        chunks = list(range(32)) + ([32, 33] if g.CTXQ else [])
        for n in chunks:
            ktok = n * 128 if n < 32 else NTOK + (n - 32) * 128
            qtok = n * 128 if n < 32 else NOWN + (n - 32) * 128
            kt, bk, vt, bv = load_kv(ktok)
            qt, bq = qr_.get()
            S.dma("sp", qt[:], qrv[:, :, qtok:qtok + 128], writes=[bq])
            gt_, bg = gr.get()
            S.dma("sp", gt_[:, 0, :], g.GF[qtok:qtok + 128, :], writes=[bg])
            S.dma("sp", gt_[:, 1, :], g.GB[qtok:qtok + 128, :], writes=[bg])
            p, bp = pA.get()
            for h in range(4):
                S.mm([bk, bq], [bp], True, out=p[:, h * 128:(h + 1) * 128], lhsT=kt[:, h, :], rhs=qt[:, h, :], start=True, stop=True)
            A, bA = Ar.get()
            for dr in range(2):
                S.op("dve", "tensor_tensor", reads=[bp, bmk], writes=[bA], out=A[:, dr, :], in0=p[:, :],
                     in1=mk[:, dr].rearrange("p h i -> p (h i)"), op=ALU.mult)
            qw_, bqw_ = qwr.get()
            for dr in range(2):
                S.op("dve", "tensor_tensor", reads=[bq, bqw], writes=[bqw_], out=qw_[:, dr].rearrange("p t i -> p (t i)"),
                     in0=qt[:].rearrange("p t i -> p (t i)"), in1=qw[:64, dr].rearrange("p t i -> p (t i)"), op=ALU.mult)
            p2, bp2 = pA.get()
            for dr in range(2):
                for h in range(4):
                    col = (dr * 4 + h) * 64
                    S.mm([bA, bv], [bp2], False, out=p2[:, col:col + 64], lhsT=A[:, dr, h * 128:(h + 1) * 128],
                         rhs=vt[:, h * 64:(h + 1) * 64], start=True, stop=False)
                    S.mm([bqw_, bSB], [bp2], True, out=p2[:, col:col + 64], lhsT=qw_[:, dr, h, :],
                         rhs=SB_[:64, n, dr, h * 64:(h + 1) * 64], start=False, stop=True)
            sq, bsq = sqr.get()
            S.op("act", "activation", reads=[bp2], writes=[bsq], out=sq[:], in_=p2[:], func=AF.Square)
            ss, bss = ssr.get()
            S.op("dve", "tensor_reduce", reads=[bsq], writes=[bss], out=ss[:], in_=sq[:].rearrange("p (g d) -> p g d", d=64),
                 axis=AX.X, op=ALU.add)
            rstd_from_ps(g, ss, bss, ss, bss, 64.0, 8)
            t, bt = tr_.get()
            for dr in range(2):
                for h in range(4):
                    col = (dr * 4 + h) * 64
                    _ts(S, t[:, dr, h * 64:(h + 1) * 64], p2[:, col:col + 64], ss[:, dr * 4 + h: dr * 4 + h + 1], [bp2, bss], [bt])
            S.op("dve", "tensor_tensor", reads=[bt, bg], writes=[bt], out=t[:].rearrange("p a b -> p (a b)"),
                 in0=t[:].rearrange("p a b -> p (a b)"), in1=gt_[:].rearrange("p a b -> p (a b)"), op=ALU.mult)
            y, by = yo.get()
            S.op("dve", "tensor_tensor", reads=[bt], writes=[by], out=y[:], in0=t[:, 0, :], in1=t[:, 1, :], op=ALU.add)
            S.dma("sp", g.YRET[qtok:qtok + 128, :], y[:], reads=[by], writes=[db["YRET"]])
        S.barrier()


def phase_fourier(g):
    nc, S, db = g.nc, g.S, g.db
    pA = SubRing(g.psb)
    with ExitStack() as es:
        ftb, bftb = sb(nc, es, "ftb", [64, 128, 256], BF16)
        cst = Caster(g, es)
        fv = g.ft.rearrange("c (r f) -> c r f", f=256)
        for r0 in range(0, 128, 8):
            cst.load(ftb[:, r0:r0 + 8, :], fv[:, r0:r0 + 8, :], bftb, 64, [8, 256])
        xr = Ring(nc, es, "fx", [64, 8, 512], BF16, 2)
        bo = Ring(nc, es, "fbo", [128, 8, 256], BF16, 2)
        xv = g.XCS[0:NTOK, :].rearrange("(c r) f -> c r f", c=64)
        for r0 in range(0, 128, 8):
            xt, bx = xr.get()
            S.dma("sp", xt[:], xv[:, r0:r0 + 8, :], writes=[bx])
            o, bob = bo.get()
            for rp in range(4):
                p, bp = pA.get()
                for z in range(2):
                    r = rp * 2 + z
                    S.mm([bftb, bx], [bp], False, out=p[:, z * 256:(z + 1) * 256], lhsT=ftb[:, r0 + r, 0:128], rhs=xt[:, r, 0:256],
                         start=True, stop=False)
                    S.mm([bftb, bx], [bp], True, out=p[:, z * 256:(z + 1) * 256], lhsT=ftb[:, r0 + r, 128:256], rhs=xt[:, r, 256:512],
                         start=False, stop=True)
                S.op("act", "copy", reads=[bp], writes=[bob], out=o[:, rp * 2:rp * 2 + 2, :].rearrange("p a b -> p (a b)"), in_=p[:, :])
            S.dma("sp", g.BP[:, r0:r0 + 8, :], o[:], reads=[bob], writes=[db["BP"]])
        S.barrier()
    with ExitStack() as es:
        R, bR = sb(nc, es, "fR", [128, 128, 256], BF16)
        bpv = g.BP.rearrange("p r c -> r p c")
        for p0 in range(0, 128, 8):
            S.dma("sp", R[:, p0:p0 + 8, :], bpv[:, p0:p0 + 8, :], reads=[db["BP"]], writes=[bR])
        c3, bc3 = sb(nc, es, "c3", [128, 2, 64], BF16)
        cst = Caster(g, es)
        cst.load(c3[:, 0, :], g.c3, bc3, 128, [64])
        cst.load(c3[:, 1, :], g.s3n, bc3, 128, [64])
        Y, bY = sb(nc, es, "fY", [64, 64, 256], BF16)
        for i in range(32):
            p, bp = pA.get()
            S.mm([bR, bc3], [bp], False, out=p[:64, :], lhsT=c3[:, 0, :], rhs=R[:, 2 * i:2 * i + 2, :].rearrange("p a b -> p (a b)"),
                 start=True, stop=False)
            S.mm([bR, bc3], [bp], True, out=p[:64, :], lhsT=c3[:, 1, :], rhs=R[:, 64 + 2 * i:64 + 2 * i + 2, :].rearrange("p a b -> p (a b)"),
                 start=False, stop=True)
            S.op("act", "copy", reads=[bp], writes=[bY], out=Y[:, 2 * i:2 * i + 2, :].rearrange("p a b -> p (a b)"), in_=p[:64, :])
        yv = g.YF[0:NOWN, :].rearrange("(k1 k0) c -> k1 k0 c", k0=64)
        for q in range(4):
            S.dma("sp", yv[:, q * 16:(q + 1) * 16, :], Y[:, q * 16:(q + 1) * 16, :], reads=[bY], writes=[db["YF"]])
        if g.CTXQ:
            ct, bct = sb(nc, es, "fct", [128, 2, 2, 256], BF16)
            cst.load(ct[:, 0], g.c256, bct, 128, [2, 256])
            cst.load(ct[:, 1], g.s256n, bct, 128, [2, 256])
            xc, bxc = sb(nc, es, "fxc", [128, 2, 512], BF16)
            S.dma("sp", xc[:], g.XCS[NTOK:NTOK + NCTX, :].rearrange("(t p) f -> p t f", p=128), writes=[bxc])
            for kt in range(2):
                p, bp = pA.get()
                n = 0
                for z in range(2):
                    for tt in range(2):
                        S.mm([bct, bxc], [bp], n == 3, out=p[:, 0:256], lhsT=ct[:, z, tt, kt * 128:(kt + 1) * 128],
                             rhs=xc[:, tt, z * 256:(z + 1) * 256], start=(n == 0), stop=(n == 3))
                        n += 1
                o, bo_ = sb(nc, es, "fco%d" % kt, [128, 256], BF16)
                S.op("act", "copy", reads=[bp], writes=[bo_], out=o[:], in_=p[:, 0:256])
                S.dma("sp", g.YF[NOWN + kt * 128: NOWN + (kt + 1) * 128, :], o[:], reads=[bo_], writes=[db["YF"]])
        S.barrier()


def phase_attn(g, kind):
    nc, S, db = g.nc, g.S, g.db
    mla = (kind == "mla")
    scale = (96.0 ** -0.5) if mla else 0.125
    with ExitStack() as es:
        Sr = SubRing(g.psb[0:5])
        Ot, bO = g.psb[5]
        Bt, bB = g.psb[6]
        if mla:
            NKEY = g.NKT
            KD = 96
            KT, bKT = sb(nc, es, "aKT", [96, 4, NKEY], BF16)
            for h in range(4):
                for c0 in range(0, NKEY, 2112):
                    S.dma("sp", KT[:, h, c0:c0 + 2112], g.MK[h, :, c0:c0 + 2112], writes=[bKT])
            vsrc = g.MV
            segs = [(0, 0, NKEY)]
        else:
            NKEY = 5120 + NCTX
            KD = 64
            KT, bKT = sb(nc, es, "aKT", [64, 4, NKEY], BF16)
            kv_ = g.NK.rearrange("(h d) t -> d h t", d=64)
            segs = [(0, 7680, 512), (512, 0, 4096), (4608, 4096, 512), (5120, NTOK, NCTX)]
            for (d0, s0, n) in segs:
                S.dma("sp", KT[:, :, d0:d0 + n], kv_[:, :, s0:s0 + n], writes=[bKT])
            vsrc = g.NV
        NT = NKEY // 128
        V, bV = sb(nc, es, "aV", [128, NT, 4, 65], BF16)
        S.op("dve", "memset", writes=[bV], ap=V[:].rearrange("p t h d -> p (t h d)"), constant=1.0)
        with ExitStack() as es2:
            vp, bvp = sb(nc, es2, "aVp", [128, NT, 256], BF16)
            for (d0, s0, n) in segs:
                for c0 in range(0, n, 1024):
                    m = min(1024, n - c0)
                    S.dma("sp", vp[:, (d0 + c0) // 128:(d0 + c0 + m) // 128, :],
                          vsrc[s0 + c0:s0 + c0 + m, :].rearrange("(t p) f -> p t f", p=128), writes=[bvp])
            for t0 in range(0, NT, 8):
                t1 = min(NT, t0 + 8)
                S.op("dve", "tensor_copy", reads=[bvp], writes=[bV], out=V[:, t0:t1, :, 0:64],
                     in_=vp[:, t0:t1, :].rearrange("p t (h d) -> p t h d", d=64))
            S.barrier()
        onef, bonef = sb(nc, es, "aone", [128, 64], F32)
        S.op("dve", "memset", writes=[bonef], ap=onef[:], constant=1.0)
        sel64, bsel64 = sb(nc, es, "asel64", [128, 64], F32)
        _ts(S, sel64[:], onef[:], g.identf[:, 64:65], [bonef, g.bidentf], [bsel64])
        qr_ = Ring(nc, es, "aq", [128, 4, 512], BF16, 2)
        Pr = Ring(nc, es, "aP", [128, 512], BF16, 4)
        tmpr = Ring(nc, es, "atmp", [128, 512], F32, 3)
        recr = Ring(nc, es, "arec", [128, 512], F32, 2)
        for (t_, b_) in recr.t:
            S.op("dve", "memset", writes=[b_], ap=t_[:], constant=0.0)
        bsr = Ring(nc, es, "abs", [64, 512], F32, 2)
        yr = Ring(nc, es, "ay", [64, 512], BF16, 3)
        nabr = Ring(nc, es, "anab", [128, 8, 512], BF16, 2) if not mla else None
        nblk = 8 + (1 if g.CTXQ else 0)
        for blk in range(nblk):
            isctx = (blk == 8)
            W = 256 if isctx else 512
            q0 = blk * 512 if not isctx else NOWN
            qt, bq = qr_.get()
            if mla:
                for h in range(4):
                    S.dma("sp", qt[:96, h, :W], g.MQ[h, :, q0:q0 + W], writes=[bq])
            else:
                S.dma("sp", qt[:64, :, :W], g.NQ_.rearrange("(h d) t -> d h t", d=64)[:, :, q0:q0 + W], writes=[bq])
            if isctx:
                ktiles = [(NT - 2, None), (NT - 1, None)]
            elif mla:
                ktiles = [(t, None) for t in range(NT)]
            else:
                ktiles = [(4 * blk + 2 + bt, bt) for bt in range(8)] + [(NT - 2, None), (NT - 1, None)]
                ty = 0 if blk == 0 else (2 if blk == 7 else 1)
            for h in range(4):
                a, tl = h % 2, h // 2
                if (not mla) and (not isctx):
                    nb, bnb = nabr.get()
                    S.dma("sp", nb[:], g.nab[ty, h].rearrange("t p q -> p t q"), writes=[bnb])
                for i, (kt, bt) in enumerate(ktiles):
                    ps, bps = Sr.get()
                    if mla:
                        S.mm([bKT, bq], [bps], True, out=ps[:, :W], lhsT=KT[:96, h, kt * 128:(kt + 1) * 128], rhs=qt[:96, h, :W],
                             start=True, stop=True)
                    else:
                        S.mm([bKT, bq], [bps], True, out=ps[:, :W], lhsT=KT[:64, h, kt * 128:(kt + 1) * 128],
                             rhs=qt[:64, h, :W], start=True, stop=True)
                    P, bP = Pr.get()
                    if bt is None:
                        S.op("act", "activation", reads=[bps], writes=[bP], out=P[:, :W], in_=ps[:, :W], func=AF.Exp, scale=scale)
                    else:
                        tm_, btm = tmpr.get()
                        _ts(S, tm_[:, :W], ps[:, :W], scale, [bps], [btm])
                        S.op("dve", "tensor_tensor", reads=[btm, bnb], writes=[btm], out=tm_[:, :W], in0=tm_[:, :W], in1=nb[:, bt, :W], op=ALU.add)
                        S.op("act", "activation", reads=[btm], writes=[bP], out=P[:, :W], in_=tm_[:, :W], func=AF.Exp)
                    last = (i == len(ktiles) - 1)
                    S.mm([bV, bP], [bO], last, out=Ot[:65, :W], lhsT=V[:, kt, h, :], rhs=P[:, :W], start=(i == 0), stop=last)
                rec, brec = recr.get()
                S.op("dve", "reciprocal", reads=[bO], writes=[brec], out=rec[64:65, :W], in_=Ot[64:65, :W])
                S.mm([brec, bsel64], [bB], True, out=Bt[:64, :W], lhsT=sel64[:, :], rhs=rec[:, :W], start=True, stop=True)
                bs_, bbs = bsr.get()
                S.op("act", "copy", reads=[bB], writes=[bbs], out=bs_[:, :W], in_=Bt[:64, :W])
                y, by = yr.get()
                S.op("dve", "tensor_tensor", reads=[bO, bbs], writes=[by], out=y[:, :W], in0=Ot[:64, :W], in1=bs_[:, :W], op=ALU.mult)
                dst = g.YMLA if mla else g.YNA
                S.dma("sp", dst[h, :, q0:q0 + W], y[:, :W], reads=[by], writes=[db["YMLA" if mla else "YNA"]])
        S.barrier()


def precast(g, es, dst, src, nk, ncols):
    S = g.S
    sv = src.rearrange("(k p) n -> p k n", p=128)
    dv = dst.rearrange("(k p) n -> p k n", p=128)
    for k in range(nk):
        for c0 in range(0, ncols, 2048):
            c1 = min(ncols, c0 + 2048)
            st, bst = g.pc_st.get()
            S.dma("sp", st[:, :c1 - c0], sv[:, k, c0:c1], writes=[bst])
            o, bo = g.pc_o.get()
            eng = "dve"
            g.pc_i += 1
            S.op(eng, "tensor_copy", reads=[bst], writes=[bo], out=o[:, :c1 - c0], in_=st[:, :c1 - c0])
            S.dma("sp", dv[:, k, c0:c1], o[:, :c1 - c0], reads=[bo], writes=[g.db["WBF"]])


def phase_out(g):
    nc, S, db = g.nc, g.S, g.db
    L = g.L
    E = 1 if L == 0 else 8
    F = 2816 if L == 0 else 3584
    NJ = F // 128
    g.WG = nc.dram_tensor("WGb", [E, D, F], BF16).ap()
    g.WU = nc.dram_tensor("WUb", [E, D, F], BF16).ap()
    g.WD = nc.dram_tensor("WDb", [E, F, D], BF16).ap()
    db["WBF"] = Buf()
    with ExitStack() as es:
        g.pc_st = Ring(nc, es, "pcs", [128, 2048], F32, 2)
        g.pc_o = Ring(nc, es, "pco", [128, 2048], BF16, 2)
        g.pc_i = 0
        for e in range(E):
            wg = g.w_g if L == 0 else g.w_g[e]
            wu = g.w_u if L == 0 else g.w_u[e]
            wd = g.w_d if L == 0 else g.w_d[e]
            precast(g, es, g.WG[e], wg, 8, F)
            precast(g, es, g.WU[e], wu, 8, F)
            precast(g, es, g.WD[e], wd, NJ, D)
        S.barrier()
    with ExitStack() as es:
        wbr, bwbr = sb(nc, es, "wbr", [128, 2, 2, D], BF16)
        wbh, bwbh = sb(nc, es, "wbh", [64, 2, 4, D], BF16)
        wout, bwout = sb(nc, es, "wout", [128, 8, D], BF16)
        if L == 1:
            rtf, brtf = sb(nc, es, "rtf", [128, 8, 8], F32)
            S.dma("sp", rtf[:], wview(g.router), writes=[brtf])
            sel, bsel = sb(nc, es, "sel", [8, D], F32)
            S.dma("sp", sel[:], g.sel, writes=[bsel])
        with ExitStack() as es2:
            cst = Caster(g, es2)
            for br in range(2):
                for k in range(2):
                    for c0 in (0, 512):
                        cst.load(wbr[:, br, k, c0:c0 + 512], g.w_branch[br, k * 128:(k + 1) * 128, c0:c0 + 512], bwbr, 128, [512])
                for h in range(4):
                    for c0 in (0, 512):
                        cst.load(wbh[:, br, h, c0:c0 + 512], g.w_branch[2 + br, h * 64:(h + 1) * 64, c0:c0 + 512], bwbh, 64, [512])
            wov = wview(g.w_out)
            for c0 in range(0, D, 256):
                cst.load(wout[:, :, c0:c0 + 256], wov[:, :, c0:c0 + 256], bwout, 128, [8, 256])
            S.barrier()
        xr = Ring(nc, es, "ox", [128, 8, 512], F32, 1)
        sqm = Ring(nc, es, "osq", [128, 8, 512], BF16, 1)
        h2r = Ring(nc, es, "oh2", [128, 8, 512], BF16, 1)
        hidr = Ring(nc, es, "ohid", [128, NJ, 512], BF16, 1)
        gtr = Ring(nc, es, "ogt", [128, 4, 512], BF16, 2 if L == 0 else 1)
        yTr = Ring(nc, es, "oyT", [128, 2, 2, 512], BF16, 1)
        ytr = Ring(nc, es, "oyt", [128, 4, 256], BF16, 2)
        yhr = Ring(nc, es, "oyh", [64, 2, 4, 512], BF16, 1)
        maccr = Ring(nc, es, "omacc", [128, 512], F32, 2)
        tmpr = Ring(nc, es, "otmp", [128, 512], F32, 3)
        wgr = Ring(nc, es, "owg", [128, 8, 256], BF16, 2)
        wur = Ring(nc, es, "owu", [128, 8, 256], BF16, 2)
        wdr = Ring(nc, es, "owd", [128, 2, 512], BF16, 2)
        if L == 1:
            yacc, byacc = sb(nc, es, "yacc", [128, 8, 512], F32)
            gbc, bgbc = sb(nc, es, "gbc", [128, 8, 512], BF16)
            lgr = Ring(nc, es, "olg", [128, 8], F32, 2)
            smr = Ring(nc, es, "osm", [128, 8], F32, 2)
            mkr = Ring(nc, es, "omk", [128, 3, 8], F32, 2)
            gTs, bgTs = sb(nc, es, "gTs", [8, 512], F32)
        pR = SubRing(g.psb[0:4])
        acc = g.psb[4:8]
        rsd, brsd = sb(nc, es, "orsd", [128, 512], F32)
        xv = g.xT.rearrange("(c p) t -> p c t", p=128)
        xov = g.xoT.rearrange("(c p) t -> p c t", p=128)
        gtv = g.GT.rearrange("(b m p) q -> p b m q", p=128, m=8)
        nblk = 8 + (1 if g.CTXQ else 0)
        for blk in range(nblk):
            isctx = (blk == 8)
            W = 256 if isctx else 512
            q0 = blk * 512 if not isctx else NOWN
            t0 = blk * 512 if not isctx else NTOK
            w = 1 if isctx else 0
            NT = W // 128
            xt, bx = xr.get()
            S.dma("sp", xt[:, :, :W], xv[:, :, t0:t0 + W], writes=[bx])
            yT, byT = yTr.get()
            for br, src in enumerate((g.YRET, g.YF)):
                yt, byt = ytr.get()
                S.dma("sp", yt[:, :NT, :], src[q0:q0 + W, :].rearrange("(t p) f -> p t f", p=128), writes=[byt])
                for c in range(2):
                    p, bp = pR.get()
                    for tt in range(NT):
                        S.mm([byt, g.bident], [bp], True, out=p[:, tt * 128:(tt + 1) * 128], lhsT=yt[:, tt, c * 128:(c + 1) * 128],
                             rhs=g.identb[:], start=True, stop=True)
                    S.op("act", "copy", reads=[bp], writes=[byT], out=yT[:, br, c, :W], in_=p[:, :W])
            yh, byh = yhr.get()
            for br, src in enumerate((g.YNA, g.YMLA)):
                S.dma("sp", yh[:, br, :, :W], src[:, :, q0:q0 + W].rearrange("h d q -> d h q"), writes=[byh])
            mT, bmT = sqm.get()
            for m in range(8):
                gt_, bgt = gtr.get()
                S.dma("sp", gt_[:, :, :W], gtv[:, :, m, q0:q0 + W], writes=[bgt])
                mac, bmac = maccr.get()
                for br in range(4):
                    p, bp = pR.get()
                    if br < 2:
                        for k in range(2):
                            S.mm([bwbr, byT], [bp], k == 1, out=p[:, :W], lhsT=wbr[:, br, k, m * 128:(m + 1) * 128], rhs=yT[:, br, k, :W],
                                 start=(k == 0), stop=(k == 1))
                    else:
                        for h in range(4):
                            S.mm([bwbh, byh], [bp], h == 3, out=p[:, :W], lhsT=wbh[:, br - 2, h, m * 128:(m + 1) * 128], rhs=yh[:, br - 2, h, :W],
                                 start=(h == 0), stop=(h == 3))
                    if br == 0:
                        S.op("dve", "tensor_tensor", reads=[bp, bgt], writes=[bmac], out=mac[:, :W], in0=p[:, :W], in1=gt_[:, 0, :W], op=ALU.mult)
                    else:
                        tm_, btm = tmpr.get()
                        S.op("dve", "tensor_tensor", reads=[bp, bgt], writes=[btm], out=tm_[:, :W], in0=p[:, :W], in1=gt_[:, br, :W], op=ALU.mult)
                        if br < 3:
                            S.op("dve", "tensor_tensor", reads=[btm, bmac], writes=[bmac], out=mac[:, :W], in0=mac[:, :W], in1=tm_[:, :W], op=ALU.add)
                        else:
                            S.op("dve", "tensor_tensor", reads=[btm, bmac], writes=[bmT], out=mT[:, m, :W], in0=mac[:, :W], in1=tm_[:, :W], op=ALU.add)
            for m in range(8):
                p, bp = pR.get()
                for k in range(8):
                    S.mm([bwout, bmT], [bp], k == 7, out=p[:, :W], lhsT=wout[:, k, m * 128:(m + 1) * 128], rhs=mT[:, k, :W], start=(k == 0), stop=(k == 7))
                tm_, btm = tmpr.get()
                _ts(S, tm_[:, :W], p[:, :W], g.mod[:, w * 48 + 16 + m: w * 48 + 17 + m], [bp, g.bmod], [btm])
                S.op("dve", "tensor_tensor", reads=[btm, bx], writes=[bx], out=xt[:, m, :W], in0=xt[:, m, :W], in1=tm_[:, :W], op=ALU.add)
            sq, bsq = sqm.get()
            S.op("act", "activation", reads=[bx], writes=[bsq], out=sq[:, :, :W], in_=xt[:, :, :W], func=AF.Square)
            p, bp = pR.get()
            for k in range(8):
                S.mm([bsq, g.bones], [bp], k == 7, out=p[:, :W], lhsT=g.ones[:], rhs=sq[:, k, :W], start=(k == 0), stop=(k == 7))
            rs_, brs = rsd, brsd
            rstd_from_ps(g, p, bp, rs_, brs, 1024.0, W)
            h2, bh2 = h2r.get()
            if L == 1:
                lps = [pR.get() for _ in range(NT)]
            for c in range(8):
                t1, bt1 = tmpr.get()
                S.op("dve", "tensor_tensor", reads=[bx, brs], writes=[bt1], out=t1[:, :W], in0=xt[:, c, :W], in1=rs_[:, :W], op=ALU.mult)
                if L == 0:
                    S.op("act", "activation", reads=[bt1, g.bam, g.bmod], writes=[bh2], out=h2[:, c, :W], in_=t1[:, :W], func=AF.Identity,
                         scale=g.am[:, w * 16 + 8 + c: w * 16 + 9 + c], bias=g.mod[:, w * 48 + 24 + c: w * 48 + 25 + c])
                else:
                    S.op("act", "activation", reads=[bt1, g.bam, g.bmod], writes=[bt1], out=t1[:, :W], in_=t1[:, :W], func=AF.Identity,
                         scale=g.am[:, w * 16 + 8 + c: w * 16 + 9 + c], bias=g.mod[:, w * 48 + 24 + c: w * 48 + 25 + c])
                    S.op("dve", "tensor_copy", reads=[bt1], writes=[bh2], out=h2[:, c, :W], in_=t1[:, :W])
                    for tt in range(NT):
                        S.mm([bt1, brtf], [lps[tt][1]], (c == 7) or (tt == NT - 1), out=lps[tt][0][:, 0:8], lhsT=t1[:, tt * 128:(tt + 1) * 128], rhs=rtf[:, c, :],
                             start=(c == 0), stop=(c == 7))
            if L == 1:
                pT, bpT = acc[0]
                for tt in range(NT):
                    lp, blp = lps[tt]
                    lg, blg = lgr.get()
                    S.op("act", "copy", reads=[blp], writes=[blg], out=lg[:], in_=lp[:, 0:8])
                    sm, bsm = smr.get()
                    mk_, bmk_ = mkr.get()
                    S.op("dve", "tensor_reduce", reads=[blg], writes=[bsm], out=sm[:, 0:1], in_=lg[:], axis=AX.X, op=ALU.max)
                    _ts(S, sm[:, 7:8], sm[:, 0:1], -1.0, [bsm], [bsm])
                    S.op("act", "activation", reads=[blg, bsm], writes=[bmk_], out=mk_[:, 2, :], in_=lg[:], func=AF.Identity, bias=sm[:, 7:8])
                    S.op("dve", "tensor_scalar", reads=[bmk_], writes=[bmk_], out=mk_[:, 0, :], in0=mk_[:, 2, :], scalar1=0.0, scalar2=0.0,
                         op0=ALU.is_ge, op1=ALU.add)
                    S.op("dve", "scalar_tensor_tensor", reads=[bmk_, blg], writes=[bmk_], out=mk_[:, 2, :], in0=mk_[:, 0, :], scalar=-1e30,
                         in1=lg[:], op0=ALU.mult, op1=ALU.add)
                    S.op("dve", "tensor_reduce", reads=[bmk_], writes=[bsm], out=sm[:, 1:2], in_=mk_[:, 2, :], axis=AX.X, op=ALU.max)
                    _ts(S, sm[:, 7:8], sm[:, 1:2], -1.0, [bsm], [bsm])
                    S.op("act", "activation", reads=[bmk_, bsm], writes=[bmk_], out=mk_[:, 2, :], in_=mk_[:, 2, :], func=AF.Identity, bias=sm[:, 7:8])
                    S.op("dve", "tensor_scalar", reads=[bmk_], writes=[bmk_], out=mk_[:, 1, :], in0=mk_[:, 2, :], scalar1=0.0, scalar2=0.0,
                         op0=ALU.is_ge, op1=ALU.add)
                    S.op("dve", "tensor_tensor", reads=[bsm], writes=[bsm], out=sm[:, 2:3], in0=sm[:, 1:2], in1=sm[:, 0:1], op=ALU.subtract)
                    S.op("act", "activation", reads=[bsm], writes=[bsm], out=sm[:, 3:4], in_=sm[:, 2:3], func=AF.Exp)
                    S.op("dve", "tensor_scalar", reads=[bsm], writes=[bsm], out=sm[:, 4:5], in0=sm[:, 3:4], scalar1=1.0, scalar2=0.0,
                         op0=ALU.add, op1=ALU.add)
                    S.op("dve", "reciprocal", reads=[bsm], writes=[bsm], out=sm[:, 5:6], in_=sm[:, 4:5])
                    S.op("dve", "tensor_tensor", reads=[bsm], writes=[bsm], out=sm[:, 6:7], in0=sm[:, 3:4], in1=sm[:, 5:6], op=ALU.mult)
                    _ts(S, mk_[:, 0, :], mk_[:, 0, :], sm[:, 5:6], [bmk_, bsm], [bmk_])
                    _ts(S, mk_[:, 1, :], mk_[:, 1, :], sm[:, 6:7], [bmk_, bsm], [bmk_])
                    S.op("dve", "tensor_tensor", reads=[bmk_], writes=[bmk_], out=mk_[:, 0, :], in0=mk_[:, 0, :], in1=mk_[:, 1, :], op=ALU.add)
                    S.mm([bmk_, g.bidentf], [bpT], True, out=pT[:8, tt * 128:(tt + 1) * 128], lhsT=mk_[:, 0, :], rhs=g.identf[:], start=True, stop=True)
                S.op("act", "copy", reads=[bpT], writes=[bgTs], out=gTs[:, :W], in_=pT[:8, :W])
                for e in range(8):
                    p, bp = pR.get()
                    S.mm([bgTs, bsel], [bp], True, out=p[:, :W], lhsT=sel[:, e * 128:(e + 1) * 128], rhs=gTs[:, :W], start=True, stop=True)
                    S.op("act", "copy", reads=[bp], writes=[bgbc], out=gbc[:, e, :W], in_=p[:, :W])
            for e in range(E):
                hid, bhid = hidr.get()
                wgv = g.WG[e].rearrange("(k p) n -> p k n", p=128)
                wuv_ = g.WU[e].rearrange("(k p) n -> p k n", p=128)
                wdv = g.WD[e].rearrange("(j p) n -> p j n", p=128)
                for jp in range(NJ // 2):
                    wg_, bwg = wgr.get()
                    wu_, bwu = wur.get()
                    S.dma("sp", wg_[:], wgv[:, :, jp * 256:(jp + 1) * 256], reads=[db["WBF"]], writes=[bwg])
                    S.dma("sp", wu_[:], wuv_[:, :, jp * 256:(jp + 1) * 256], reads=[db["WBF"]], writes=[bwu])
                    for jj in range(2):
                        j = jp * 2 + jj
                        pg, bpg = pR.get()
                        pu, bpu = pR.get()
                        for k in range(8):
                            S.mm([bwg, bh2], [bpg], k == 7, out=pg[:, :W], lhsT=wg_[:, k, jj * 128:(jj + 1) * 128], rhs=h2[:, k, :W], start=(k == 0), stop=(k == 7))
                        for k in range(8):
                            S.mm([bwu, bh2], [bpu], k == 7, out=pu[:, :W], lhsT=wu_[:, k, jj * 128:(jj + 1) * 128], rhs=h2[:, k, :W], start=(k == 0), stop=(k == 7))
                        sg, bsg = tmpr.get()
                        S.op("act", "activation", reads=[bpg], writes=[bsg], out=sg[:, :W], in_=pg[:, :W], func=AF.Silu)
                        if L == 0:
                            S.op("dve", "tensor_tensor", reads=[bsg, bpu], writes=[bhid], out=hid[:, j, :W], in0=sg[:, :W], in1=pu[:, :W], op=ALU.mult)
                        else:
                            S.op("dve", "tensor_tensor", reads=[bsg, bpu], writes=[bsg], out=sg[:, :W], in0=sg[:, :W], in1=pu[:, :W], op=ALU.mult)
                            S.op("dve", "tensor_tensor", reads=[bsg, bgbc], writes=[bhid], out=hid[:, j, :W], in0=sg[:, :W], in1=gbc[:, e, :W], op=ALU.mult)
                for half in range(2):
                    for jp in range(NJ // 2):
                        wd_, bwd = wdr.get()
                        S.dma("sp", wd_[:], wdv[:, jp * 2:jp * 2 + 2, half * 512:(half + 1) * 512], reads=[db["WBF"]], writes=[bwd])
                        for jj in range(2):
                            j = jp * 2 + jj
                            for mi in range(4):
                                S.mm([bwd, bhid], [acc[mi][1]], (j == NJ - 1) or (jj == 1 and mi == 3), out=acc[mi][0][:, :W], lhsT=wd_[:, jj, mi * 128:(mi + 1) * 128],
                                     rhs=hid[:, j, :W], start=(j == 0), stop=(j == NJ - 1))
                    for mi in range(4):
                        m = half * 4 + mi
                        pa, bpa = acc[mi]
                        if L == 0:
                            tm_, btm = tmpr.get()
                            _ts(S, tm_[:, :W], pa[:, :W], g.mod[:, w * 48 + 40 + m: w * 48 + 41 + m], [bpa, g.bmod], [btm])
                            S.op("dve", "tensor_tensor", reads=[btm, bx], writes=[bx], out=xt[:, m, :W], in0=xt[:, m, :W], in1=tm_[:, :W], op=ALU.add)
                        elif e == 0:
                            S.op("act", "copy", reads=[bpa], writes=[byacc], out=yacc[:, m, :W], in_=pa[:, :W])
                        else:
                            S.op("dve", "tensor_tensor", reads=[bpa, byacc], writes=[byacc], out=yacc[:, m, :W], in0=pa[:, :W], in1=yacc[:, m, :W], op=ALU.add)
            if L == 1:
                for m in range(8):
                    tm_, btm = tmpr.get()
                    _ts(S, tm_[:, :W], yacc[:, m, :W], g.mod[:, w * 48 + 40 + m: w * 48 + 41 + m], [byacc, g.bmod], [btm])
                    S.op("dve", "tensor_tensor", reads=[btm, bx], writes=[bx], out=xt[:, m, :W], in0=xt[:, m, :W], in1=tm_[:, :W], op=ALU.add)
                sq, bsq = sqm.get()
                S.op("act", "activation", reads=[bx], writes=[bsq], out=sq[:, :, :W], in_=xt[:, :, :W], func=AF.Square)
                p, bp = pR.get()
                for k in range(8):
                    S.mm([bsq, g.bones], [bp], k == 7, out=p[:, :W], lhsT=g.ones[:], rhs=sq[:, k, :W], start=(k == 0), stop=(k == 7))
                rs_, brs = rsd, brsd
                rstd_from_ps(g, p, bp, rs_, brs, 1024.0, W)
                for c in range(8):
                    _ts(S, xt[:, c, :W], xt[:, c, :W], g.am[:, 32 + c: 33 + c], [bx, g.bam], [bx])
                    S.op("dve", "tensor_tensor", reads=[bx, brs], writes=[bx], out=xt[:, c, :W], in0=xt[:, c, :W], in1=rs_[:, :W], op=ALU.mult)
            S.dma("sp", xov[:, :, q0:q0 + W], xt[:, :, :W], reads=[bx], writes=[db["xo"]])
        S.barrier()
```
